# Optimizing a Trainium2 kernel written in Bass

```python
import jax, jax.numpy as jnp
from jax import lax
import numpy as np

D_MODEL = 4096
BATCH = 4
SEQ = 4096
DEPTH = 1

MIX_WIDTH = D_MODEL
RET_HEADS = 8
RET_WIDTH = MIX_WIDTH // 2
RET_HEAD_DIM = RET_WIDTH // RET_HEADS
SGU_GROUPS = 8
SGU_WIDTH = MIX_WIDTH - RET_WIDTH
SGU_GROUP_DIM = SGU_WIDTH // SGU_GROUPS
CHUNK = 128
IN_WIDTH = 4 * RET_WIDTH + 2 * SGU_WIDTH
D_FF = 256 * ((8 * D_MODEL // 3 + 255) // 256)
ROPE_BASE = 10000.0
EPS = 1e-6
N_MOD = 9

kernel_name = "hybrid_retention_gmlp_macaron_adaln"


def rmsnorm(x, g):
    xf = x.astype(jnp.float32)
    y = xf * lax.rsqrt(jnp.mean(xf * xf, axis=-1, keepdims=True) + EPS)
    return (y * g.astype(jnp.float32)).astype(x.dtype)


def modulate(h, shift, scale):
    return h * (1.0 + scale[:, None, :]) + shift[:, None, :]


def swiglu(h, w1, w3, w2):
    return (jax.nn.silu(h @ w1) * (h @ w3)) @ w2


def rotary(t, positions):
    dh = t.shape[-1]
    half = dh // 2
    inv_freq = ROPE_BASE ** (-jnp.arange(0, half, dtype=jnp.float32) / half)
    ang = positions.astype(jnp.float32)[..., None] * inv_freq
    cos = jnp.cos(ang)[:, :, None, :]
    sin = jnp.sin(ang)[:, :, None, :]
    tf = t.astype(jnp.float32)
    t1, t2 = tf[..., :half], tf[..., half:]
    return jnp.concatenate([t1 * cos - t2 * sin, t1 * sin + t2 * cos], axis=-1)


def retention_chunkwise(q, k, v):
    b, s, h, dh = q.shape
    nc = s // CHUNK
    log_gamma = jnp.log(1.0 - 2.0 ** (-5.0 - jnp.arange(h, dtype=jnp.float32)))
    idx = jnp.arange(CHUNK)
    dist = (idx[:, None] - idx[None, :]).astype(jnp.float32)
    intra_decay = jnp.where(dist[None] >= 0,
                            jnp.exp(log_gamma[:, None, None] * jnp.maximum(dist, 0.0)[None]), 0.0)
    xi = jnp.exp(log_gamma[:, None] * (idx + 1).astype(jnp.float32))[None, :, :, None]
    zeta = jnp.exp(log_gamma[:, None] * (CHUNK - 1 - idx).astype(jnp.float32))[None, :, :, None]
    chunk_decay = jnp.exp(log_gamma * CHUNK)[None, :, None, None]

    def to_chunks(t):
        return t.reshape(b, nc, CHUNK, h, dh).transpose(1, 0, 3, 2, 4)

    qc, kc, vc = to_chunks(q), to_chunks(k * (dh ** -0.5)), to_chunks(v)

    def step(state, inp):
        qi, ki, vi = inp
        scores = jnp.einsum('bhid,bhjd->bhij', qi, ki) * intra_decay[None]
        intra = jnp.einsum('bhij,bhje->bhie', scores, vi)
        inter = jnp.einsum('bhid,bhde->bhie', qi, state) * xi
        new_state = state * chunk_decay + jnp.einsum('bhjd,bhje->bhde', ki * zeta, vi)
        return new_state, intra + inter

    state0 = jnp.zeros((b, h, dh, dh), jnp.float32)
    _, out = lax.scan(step, state0, (qc, kc, vc))
    return out.transpose(1, 0, 3, 2, 4).reshape(b, s, h, dh)


def chunked_spatial_gating(u, vs, norm_g, norm_b, w_s, b_s):
    b, s, _ = u.shape
    nc = s // CHUNK
    vg = vs.reshape(b, s, SGU_GROUPS, SGU_GROUP_DIM).astype(jnp.float32)
    mu = jnp.mean(vg, axis=-1, keepdims=True)
    var = jnp.mean(jnp.square(vg - mu), axis=-1, keepdims=True)
    vg = (vg - mu) * lax.rsqrt(var + EPS) * norm_g + norm_b
    vg = vg.reshape(b, nc, CHUNK, SGU_GROUPS, SGU_GROUP_DIM)
    causal = jnp.tril(jnp.ones((CHUNK, CHUNK), jnp.float32))
    w_masked = w_s.astype(jnp.float32) * causal[None]
    mixed = jnp.einsum('gij,bnjgd->bnigd', w_masked, vg) + b_s.T.astype(jnp.float32)[None, None, :, :, None]
    gate = mixed.reshape(b, s, SGU_WIDTH).astype(u.dtype)
    return u * gate


def setup_inputs(seed: int = 0) -> dict:
    key = jax.random.key(seed)
    ks = jax.random.split(key, 24)
    L, D = DEPTH, D_MODEL
    nrm = lambda k, shape, fan_in: jax.random.normal(k, shape, jnp.float32) * (fan_in ** -0.5)
    gain = lambda k, shape: 1.0 + 0.02 * jax.random.normal(k, shape, jnp.float32)
    x = jax.random.normal(ks[0], (BATCH, SEQ, D), jnp.float32)
    c = jax.random.normal(ks[1], (BATCH, D), jnp.float32)
    offsets = jax.random.randint(ks[2], (BATCH, 1), 0, SEQ, dtype=jnp.int32)
    positions = (jnp.arange(SEQ, dtype=jnp.int32)[None, :] + offsets).astype(jnp.int32)
    return {
        "x": x,
        "c": c,
        "positions": positions,
        "ada_w": nrm(ks[3], (L, D, N_MOD * D), D),
        "ada_b": 0.02 * jax.random.normal(ks[4], (L, N_MOD * D), jnp.float32),
        "norm_ffn1_g": gain(ks[5], (L, D)),
        "ffn1_w1": nrm(ks[6], (L, D, D_FF), D),
        "ffn1_w3": nrm(ks[7], (L, D, D_FF), D),
        "ffn1_w2": nrm(ks[8], (L, D_FF, D), D_FF),
        "norm_mix_g": gain(ks[9], (L, D)),
        "w_in": nrm(ks[10], (L, D, IN_WIDTH), D),
        "sgu_norm_g": gain(ks[11], (L, SGU_GROUPS, SGU_GROUP_DIM)),
        "sgu_norm_b": 0.02 * jax.random.normal(ks[12], (L, SGU_GROUPS, SGU_GROUP_DIM), jnp.float32),
        "sgu_w_s": nrm(ks[13], (L, SGU_GROUPS, CHUNK, CHUNK), CHUNK),
        "sgu_b_s": gain(ks[14], (L, SGU_GROUPS, CHUNK)),
        "w_out": nrm(ks[15], (L, MIX_WIDTH, D), MIX_WIDTH),
        "norm_ffn2_g": gain(ks[16], (L, D)),
        "ffn2_w1": nrm(ks[17], (L, D, D_FF), D),
        "ffn2_w3": nrm(ks[18], (L, D, D_FF), D),
        "ffn2_w2": nrm(ks[19], (L, D_FF, D), D_FF),
        "final_norm_g": gain(ks[20], (D,)),
    }


def reference(x, c, positions, ada_w, ada_b, norm_ffn1_g, ffn1_w1, ffn1_w3, ffn1_w2, norm_mix_g, w_in,
              sgu_norm_g, sgu_norm_b, sgu_w_s, sgu_b_s, w_out, norm_ffn2_g, ffn2_w1, ffn2_w3, ffn2_w2,
              final_norm_g):
    b, s, _ = x.shape
    for l in range(DEPTH):
        mod = jax.nn.silu(c) @ ada_w[l] + ada_b[l]
        (sh1, sc1, gt1, sh2, sc2, gt2, sh3, sc3, gt3) = jnp.split(mod, N_MOD, axis=-1)

        h = modulate(rmsnorm(x, norm_ffn1_g[l]), sh1, sc1)
        x = x + 0.5 * gt1[:, None, :] * swiglu(h, ffn1_w1[l], ffn1_w3[l], ffn1_w2[l])

        h = modulate(rmsnorm(x, norm_mix_g[l]), sh2, sc2)
        proj = h @ w_in[l]
        q, k, v, g, u, vs = jnp.split(proj, [RET_WIDTH, 2 * RET_WIDTH, 3 * RET_WIDTH, 4 * RET_WIDTH,
                                             4 * RET_WIDTH + SGU_WIDTH], axis=-1)
        hs = (b, s, RET_HEADS, RET_HEAD_DIM)
        qr = rotary(q.reshape(hs), positions)
        kr = rotary(k.reshape(hs), positions)
        ret = retention_chunkwise(qr, kr, v.reshape(hs).astype(jnp.float32))
        ret = ret * lax.rsqrt(jnp.mean(ret * ret, axis=-1, keepdims=True) + EPS)
        ret = ret.reshape(b, s, RET_WIDTH).astype(x.dtype) * jax.nn.silu(g)
        sgu = chunked_spatial_gating(jax.nn.gelu(u), jax.nn.gelu(vs), sgu_norm_g[l], sgu_norm_b[l],
                                     sgu_w_s[l], sgu_b_s[l])
        mixed = jnp.concatenate([ret, sgu], axis=-1) @ w_out[l]
        x = x + gt2[:, None, :] * mixed

        h = modulate(rmsnorm(x, norm_ffn2_g[l]), sh3, sc3)
        x = x + 0.5 * gt3[:, None, :] * swiglu(h, ffn2_w1[l], ffn2_w3[l], ffn2_w2[l])
    return rmsnorm(x, final_norm_g)
```

```python
import math
import numpy as np
import concourse.bass as bass
import concourse.mybir as mybir
from concourse.bass_utils import run_bass_kernel_spmd

F32 = mybir.dt.float32
BF16 = mybir.dt.bfloat16
I32 = mybir.dt.int32
AF = mybir.ActivationFunctionType
ALU = mybir.AluOpType
AX = mybir.AxisListType

EPS = 1e-6
HD = 256
CH = 128
T = 256
NB = 4
SLOT = 8192


class Prog:
    def __init__(self):
        self.q = {e: [] for e in ("pe", "act", "dve", "pool", "sp")}
        self.cnt = {e: 0 for e in self.q}
        self.res = {}
        self.waited = {e: {} for e in self.q}
        self.slot_cnt = [0] * NB
        self.sems = {}
        self.pelog = []
        self.phase = "setup"

    def _deps(self, eng, reads, writes):
        toks = []
        for r in reads:
            st = self.res.get(r)
            if st and st["w"] is not None:
                toks.append(st["w"])
        for w in writes:
            st = self.res.get(w)
            if st:
                if st["w"] is not None and not (st["w"][0] == eng and st["w"][1] == eng):
                    toks.append(st["w"])
                for t in st["r"]:
                    if not (t[0] == eng and t[1] == eng):
                        toks.append(t)
        need = {}
        for (e, sem, val) in toks:
            if e == eng and eng == "pe":
                continue
            if need.get(sem, 0) < val:
                need[sem] = val
        out = []
        for sem, val in need.items():
            if self.waited[eng].get(sem, 0) < val:
                self.waited[eng][sem] = val
                out.append((sem, val))
        return out

    def _commit(self, tok, reads, writes):
        for r in reads:
            st = self.res.setdefault(r, {"w": None, "r": []})
            st["r"].append(tok)
        for w in writes:
            self.res[w] = {"w": tok, "r": []}

    def op(self, eng, reads, writes, fn):
        waits = self._deps(eng, reads, writes)
        self.cnt[eng] += 1
        if eng in ("pe", "act", "dve"):
            sem, val, inc = eng, self.cnt[eng], 1
        elif eng == "sp":
            sem, val, inc = "sp", 16 * self.cnt[eng], 16
            if self.cnt[eng] > 1:
                prev = 16 * (self.cnt[eng] - 1)
                if self.waited[eng].get("sp", 0) < prev:
                    self.waited[eng]["sp"] = prev
                    waits.append(("sp", prev))
        elif eng == "pool":
            self.npd = getattr(self, "npd", 0) + 1
            sem, val, inc = "pd", 16 * self.npd, 16
            if self.npd > 1:
                prev = 16 * (self.npd - 1)
                if self.waited[eng].get("pd", 0) < prev:
                    self.waited[eng]["pd"] = prev
                    waits.append(("pd", prev))
        else:
            raise ValueError(eng)
        tok = (eng, sem, val)
        self.q[eng].append((waits, fn, sem, inc))
        self._commit(tok, reads, writes)

    def qdma(self, queue, reads, writes, fn):
        sem = {"sp": "sp", "act": "ad", "pool": "pd"}[queue]
        waits = self._deps(queue, reads, writes)
        self.ndma = getattr(self, "ndma", {})
        n = self.ndma.get(queue, 0) + 1
        self.ndma[queue] = n
        if n > 1:
            prev = 16 * (n - 1)
            if self.waited[queue].get(sem, 0) < prev:
                self.waited[queue][sem] = prev
                waits.append((sem, prev))
        tok = (queue, sem, 16 * n)
        self.q[queue].append((waits, fn, sem, 16))
        self._commit(tok, reads, writes)

    def spw_load(self, slot, reads, fn):
        eng = "sp"
        key = ("slot", slot)
        waits = self._deps(eng, reads, [key])
        self.slot_cnt[slot] += 1
        sem = "w%d" % slot
        tok = (eng, sem, 16 * self.slot_cnt[slot])
        self.q[eng].append((waits, fn, sem, 16))
        self._commit(tok, reads, [key])

    def spw_store(self, slot, writes, fn):
        eng = "sp"
        key = ("slot", slot)
        waits = self._deps(eng, [key], writes)
        self.bcnt = getattr(self, "bcnt", [0] * NB)
        self.bcnt[slot] += 1
        sem = "b%d" % slot
        tok = (eng, sem, 16 * self.bcnt[slot])
        self.q[eng].append((waits, fn, sem, 16))
        self._commit(tok, [key], writes)

    def wload(self, slot, fns):
        eng = "pool"
        key = ("slot", slot)
        waits = self._deps(eng, [], [key])
        sem = "w%d" % slot
        for i, fn in enumerate(fns):
            self.slot_cnt[slot] += 1
            self.q[eng].append((waits if i == 0 else [], fn, sem, 16))
        tok = (eng, sem, 16 * self.slot_cnt[slot])
        self._commit(tok, [], [key])

    def coll(self, reads, writes, fn):
        eng = "pool"
        waits = self._deps(eng, reads, writes)
        self.ncc = getattr(self, "ncc", 0) + 1
        tok = (eng, "cc", self.ncc)
        self.q[eng].append((waits, fn, "cc", 1))
        self._commit(tok, reads, writes)

    def emit(self, nc, block, semmap):
        engs = {"pe": "tensor", "act": "scalar", "dve": "vector", "pool": "gpsimd", "sp": "sync"}
        for e, name in engs.items():
            ops = self.q[e]

            def body(eng, ops=ops, e=e):
                for waits, fn, sem, inc in ops:
                    for (s, v) in waits:
                        eng.wait_ge(semmap[s], v)
                    ins = fn(eng)
                    ins.then_inc(semmap[sem], inc)
                if e == "sp":
                    eng.wait_ge(semmap["sp"], 16 * getattr(self, "ndma", {}).get("sp", 0))
            getattr(block, name)(body)


def build(cfg):
    D, DFF, H, SEQ = cfg["D"], cfg["DFF"], cfg["H"], cfg["SEQ"]
    KD = D // 128
    RW = H * HD
    INW = 6 * RW
    NT = SEQ // (2 * T)
    TC = T // CH
    NG = DFF // 256
    NF = DFF // 128
    NCOL = min(D, 1024)
    NCB = D // NCOL
    gam = [1.0 - 2.0 ** (-5.0 - h) for h in range(H)]

    nc = bass.Bass("TRN2", target_bir_lowering=False)

    def din(name, shape, dt=F32):
        return nc.dram_tensor(name, list(shape), dt, kind="ExternalInput").ap()

    xT = din("xT", [D, SEQ // 2])
    NCORE = 2 * cfg["BATCH"]
    NB_ = cfg["BATCH"]
    CW = 9 * D // 2
    NCHL = CW // 128
    cT = din("cT", [128, KD])
    posb = din("posb", [128, SEQ // 2], I32)
    post = din("post", [128, SEQ // 256], I32)
    ada_w = din("ada_w", [D, CW])
    ada_bT = din("ada_bT", [128, NCHL])
    gains = din("gains", [128, 4, KD])
    w1 = [din("f1w1", [D, DFF]), din("f2w1", [D, DFF])]
    w3 = [din("f1w3", [D, DFF]), din("f2w3", [D, DFF])]
    w2 = [din("f1w2", [DFF, D]), din("f2w2", [DFF, D])]
    w_in = din("w_in", [D, INW])
    w_out = din("w_out", [D, D])
    sgu_gb = din("sgu_gb", [128, 2, 2 * H])
    wsT = din("wsT", [128, H, 128])
    bsb = din("bsb", [128, H, 128])
    cst = din("cst", [128, 2 * H + 3, 128])
    csc = din("csc", [128, H + 1 + H * TC])
    cm = din("cm", [128, 2 + H])
    outT = nc.dram_tensor("outT", [D, SEQ // 2], F32, kind="ExternalOutput").ap()

    NPAIR = cfg["BATCH"]
    st_out = [nc.dram_tensor("st_out%d" % i, [128, H * 512], F32) for i in range(NT)]
    st_all = [nc.dram_tensor("st_all%d" % i, [256, H * 512], F32) for i in range(NT)]
    m_out = nc.dram_tensor("m_out", [128, NCHL], F32)
    m_all = nc.dram_tensor("m_all", [2 * 128, NCHL], F32)
    kvs = nc.dram_tensor("kvs", [NT * H, 128, 2 * TC * HD], BF16).ap()
    NGR = 2 * (2 * NG + ((NG + 3) // 4) * NCB) + 9 * H
    WCN = 96
    wcs = [nc.dram_tensor("wcache%d" % i, [WCN, 128, SLOT], BF16).ap() for i in range((NGR + WCN - 1) // WCN)]
    wcg = lambda gi: wcs[gi // WCN][gi % WCN]
    import os
    dummies = [nc.dram_tensor("dummy%d" % i, [WCN, 128, SLOT], BF16).ap() for i in range(int(os.environ.get("DUMMY_SCRATCH", "0")))]
    P = Prog()
    import contextlib
    es = contextlib.ExitStack()

    def sb(name, shape, dt=F32):
        return es.enter_context(nc.sbuf_tensor(name, list(shape), dt))

    def ps(name):
        return es.enter_context(nc.psum_tensor(name, [128, 512], F32))

    with es:
        x = sb("x", [128, KD, T])
        h = sb("h", [128, KD, T], BF16)
        ring = [sb("ring%d" % i, [128, SLOT], BF16) for i in range(NB)]
        mid = [sb("mid%d" % i, [128, 8, T], BF16) for i in range(2)]
        S = sb("S", [128, H, 2, HD])
        Sbf = sb("Sbf", [128, 2, HD], BF16)
        qh = sb("qh", [128, 2, T], BF16)
        kh = sb("kh", [128, 2, T], BF16)
        kvt = sb("kvt", [128, 2, TC, HD], BF16)
        ktok = kvt[:, 0]
        vtok = kvt[:, 1]
        sg = sb("sg", [128, 2, T], BF16)
        gu = sb("gu", [128, 2, T], BF16)
        sTm = sb("sTm", [128, 128], BF16)
        ret = sb("ret", [128, 2, T])
        mix = sb("mix", [128, 2, T], BF16)
        gv = sb("gv", [128, TC, HD])
        ztok = sb("ztok", [128, TC, HD], BF16)
        f1 = sb("f1", [128, 2, T])
        f2 = sb("f2", [128, 2, T])
        f1b = f1[:].rearrange("p a b -> p (a b)").bitcast(BF16).rearrange("p (u c t) -> p u c t", u=2, c=2)
        F1K = [("f1", 0), ("f1", 1)]
        f3 = sb("f3", [128, 2, T])
        sa = [sb("sa%d" % i, [128, T]) for i in range(2)]
        rbc = sb("rbc", [128, T])
        st4 = sb("st4", [128, 8])
        cosF = sb("cosF", [128, T]); sinF = sb("sinF", [128, T])
        cosT = sb("cosT", [128, TC, 128]); sinT = sb("sinT", [128, TC, 128])
        posF = sb("posF", [128, T]); posI = sb("posI", [128, T], I32)
        posTi = sb("posTi", [128, SEQ // 256], I32); posTf = sb("posTf", [128, SEQ // 256])
        ki = sb("ki", [128, T], I32)
        cst_sb = sb("cst_sb", [128, 2 * H + 3, 128])
        csc_sb = sb("csc_sb", [128, H + 1 + H * TC])
        cm_sb = sb("cm_sb", [128, 2 + H])
        R = sb("R", [128, 2, HD])
        cbuf = sb("cbuf", [128, 2 * HD])
        stg = sb("stg", [128, 2, 2 * HD])
        wsf = stg[:].rearrange("p a b -> p (a b)")[:, 0:H * 128].rearrange("p (g i) -> p g i", i=128)
        wm = sb("wm", [128, H, 128], BF16)
        bs_sb = sb("bs_sb", [128, H, 128])
        T2 = sb("T2", [128, H, 2, 128])
        gb_sb = sb("gb_sb", [128, 2, 2 * H])
        gains_sb = sb("gains_sb", [128, 4, KD])
        c_sb = sb("c_sb", [128, KD]); c_bf = sb("c_bf", [128, KD], BF16)
        adab = sb("adab", [128, NCHL])
        mod_loc = sb("mod_loc", [128, NCHL])
        mod = sb("mod", [128, 9 * KD])
        gs = sb("gs", [128, 3, KD])
        gt = sb("gt", [128, 3, KD])
        ones = sb("ones", [128, 128])
        psA = ps("psA"); psB = ps("psB"); psX = ps("psX"); psY = ps("psY")
        psO = [ps("psO0"), ps("psO1")]; psS = ps("psS"); psZ = ps("psZ")

        xi = lambda hh: cst_sb[:, hh, :]
        kz = lambda hh: cst_sb[:, H + hh, :]
        maskT = cst_sb[:, 2 * H, :]
        invf_bc = cst_sb[:, 2 * H + 1, :]

        def bc(ap, n):
            return ap.unsqueeze(1).to_broadcast([128, n, ap.shape[-1]])

        wctr = [0]
        gctr = [0]
        cache_mode = [None]

        def wload(src, n_inner, total):
            slot = wctr[0] % NB
            wctr[0] += 1
            dst = ring[slot][:, 0:total].rearrange("p (k f) -> p k f", f=n_inner)
            if cache_mode[0] is not None:
                gi = gctr[0]
                gctr[0] += 1
                assert gi < NGR
                if cache_mode[0] == "use":
                    P.spw_load(slot, [("wc", gi)],
                               lambda e, gi=gi, slot=slot, total=total: e.dma_start(out=ring[slot][:, 0:total], in_=wcg(gi)[:, 0:total]))
                    return slot, dst
            if n_inner * 4 > 4096:
                fns = [(lambda g, d=dst[:, j, :], s_=src[:, j, :]: g.dma_start(out=d, in_=s_, max_dma_last_dim=4096))
                       for j in range(total // n_inner)]
            else:
                fns = [lambda g, dst=dst, src=src: g.dma_start(out=dst, in_=src, max_dma_last_dim=4096)]
            P.wload(slot, fns)
            if cache_mode[0] == "fill":
                P.spw_store(slot, [("wc", gi)],
                            lambda e, gi=gi, slot=slot, total=total: e.dma_start(out=wcg(gi)[:, 0:total], in_=ring[slot][:, 0:total]))
            return slot, dst

        def colgran(w, c0):
            return wload(w.rearrange("(k p) f -> p k f", p=128)[:, :, c0:c0 + 256], 256, KD * 256)

        def dma(dst, src, reads, writes, q="sp"):
            P.qdma(q, reads, writes, lambda s: s.dma_start(out=dst, in_=src))

        def tt(out, a, b, op, reads, writes, eng="dve"):
            P.op(eng, reads, writes, lambda v: v.tensor_tensor(out=out, in0=a, in1=b, op=op))

        def ts(out, a, s1, s2, op0, op1, reads, writes):
            if op1 is None:
                P.op("dve", reads, writes, lambda v: v.tensor_scalar(out=out, in0=a, scalar1=s1, scalar2=None, op0=op0))
            else:
                P.op("dve", reads, writes, lambda v: v.tensor_scalar(out=out, in0=a, scalar1=s1, scalar2=s2, op0=op0, op1=op1))

        def stt(out, a, s, b, op0, op1, reads, writes):
            P.op("dve", reads, writes, lambda v: v.scalar_tensor_tensor(out=out, in0=a, scalar=s, in1=b, op0=op0, op1=op1))

        def act(out, a, func, reads, writes, bias=None, scale=None):
            kw = {}
            if bias is not None:
                kw["bias"] = bias
            if scale is not None:
                kw["scale"] = scale
            P.op("act", reads, writes, lambda e: e.activation(out=out, in_=a, func=func, **kw))

        def cp(out, a, reads, writes):
            P.op("dve", reads, writes, lambda v: v.tensor_copy(out=out, in_=a))

        def mm(groups, reads, writes):
            P.pelog.append((P.phase, str(writes), sum(len(g) for g in groups) * (2 if groups[0][0][1].dtype == F32 else 1)))
            def fn(t):
                ins = None
                for g in groups:
                    n = len(g)
                    for i, (o, l, r) in enumerate(g):
                        ins = t.matmul(o, l, r, start=(i == 0), stop=(i == n - 1))
                return ins
            P.op("pe", reads, writes, fn)

        def rstd_from(psum_ap, n, reads, out):
            act(out, psum_ap, AF.Sqrt, reads, ["rbc"], bias=eps_sb[:, 0:1], scale=1.0 / n)
            P.op("dve", ["rbc"], ["rbc"], lambda v: v.reciprocal(out=out, in_=out))

        eps_sb = sb("eps_sb", [128, 1])

        P.op("dve", [], ["ones"], lambda v: v.memset(ones[:], 1.0))
        P.op("dve", [], ["eps"], lambda v: v.memset(eps_sb[:], EPS))
        P.op("dve", [], ["S"], lambda v: v.memset(S[:], 0.0))
        dma(c_sb[:], cT, [], ["c_sb"])
        dma(adab[:], ada_bT, [], ["adab"])
        dma(gains_sb[:], gains, [], ["gains"])
        dma(gb_sb[:], sgu_gb, [], ["gb"])
        dma(wsf, wsT, [], ["stg"])
        dma(bs_sb[:], bsb, [], ["bs"])
        dma(cst_sb[:], cst, [], ["cst"])
        dma(csc_sb[:], csc, [], ["csc"])
        dma(cm_sb[:], cm, [], ["cm"])
        dma(posTi[:], post, [], ["posTi"])
        cp(posTf[:], posTi[:], ["posTi"], ["posTf"])
        act(c_bf[:], c_sb[:], AF.Silu, ["c_sb"], ["c_bf"])
        tt(wm[:], wsf, bc(maskT, H), ALU.mult, ["stg", "cst"], ["wm"])
        ones_bf = sb("ones_bf", [128, 128], BF16)
        P.op("dve", [], ["ones_bf"], lambda v: v.memset(ones_bf[:], 1.0))
        for g in range(H):
            mm([[(psS[:, 0:128], ones_bf[:], wm[:, g, :])]], ["ones_bf", "wm"], ["psS"])
            for dch in range(2):
                stt(T2[:, g, dch, :], psS[:, 0:128], gb_sb[:, 1, 2 * g + dch:2 * g + dch + 1], bs_sb[:, g, :],
                    ALU.mult, ALU.add, ["psS", "gb", "bs"], ["T2"])
        for g in range(CW // 256):
            slot, wv = colgran(ada_w, g * 256)
            groups = [[(psZ[:, 2 * g + j:2 * g + j + 1], wv[:, k, j * 128:(j + 1) * 128], c_bf[:, k:k + 1])
                       for k in range(KD)] for j in range(2)]
            mm(groups, [("slot", slot), "c_bf"], ["psZ"])
        tt(mod_loc[:], psZ[:, 0:NCHL], adab[:], ALU.add, ["psZ", "adab"], ["mod_loc"])
        dma(m_out.ap()[:, :], mod_loc[:], ["mod_loc"], ["m_out"])
        P.coll(["m_out"], ["m_all"],
               lambda g: g.collective_compute("AllGather", ALU.bypass,
                                              replica_groups=[[2 * i, 2 * i + 1] for i in range(NPAIR)],
                                              ins=[m_out.ap()[:, :]], outs=[m_all.ap()[:, :]]))
        dma(mod[:].rearrange("p (r c) -> p r c", c=NCHL), m_all.ap().rearrange("(r p) f -> p r f", p=128), ["m_all"], ["mod"])
        for i in range(3):
            stt(gs[:, i, :], mod[:, (3 * i + 1) * KD:(3 * i + 2) * KD], 1.0, gains_sb[:, i, :], ALU.add, ALU.mult,
                ["mod", "gains"], ["gsgt"])
            ts(gt[:, i, :], mod[:, (3 * i + 2) * KD:(3 * i + 3) * KD], 0.5 if i != 1 else 1.0, None, ALU.mult, None,
               ["mod"], ["gsgt"])
        sh = lambda i, k: mod[:, 3 * i * KD + k:3 * i * KD + k + 1]

        def sincos(ang, sin_o, cos_o, tmpa, tmpb, kint, key, angk, ak, bk):
            C1 = 6.28125
            C2 = 2.0 * math.pi - C1
            ts(tmpa, ang, 1.0 / (2.0 * math.pi), None, ALU.mult, None, angk, ak)
            cp(kint, tmpa, ak, ["kint"])
            cp(tmpa, kint, ["kint"], ak)
            stt(tmpb, tmpa, -C1, ang, ALU.mult, ALU.add, ak + angk, bk)
            stt(tmpb, tmpa, -C2, tmpb, ALU.mult, ALU.add, ak + bk, bk)
            ts(tmpb, tmpb, math.pi, -math.pi, ALU.min, ALU.max, bk, bk)
            act(sin_o, tmpb, AF.Sin, bk, [key + "sin"])
            ts(tmpa, tmpb, math.pi / 2, None, ALU.add, None, bk, ak)
            ts(tmpb, tmpa, math.pi, -2.0 * math.pi, ALU.is_gt, ALU.mult, ak, bk)
            tt(tmpa, tmpa, tmpb, ALU.add, ak + bk, ak)
            ts(tmpa, tmpa, math.pi, -math.pi, ALU.min, ALU.max, ak, ak)
            act(cos_o, tmpa, AF.Sin, ak, [key + "cos"])

        def norm_to_h(i):
            P.phase = "norm%d" % i
            for k2 in range(KD // 2):
                u = k2 % 2
                act(f1b[:, u], x[:, 2 * k2:2 * k2 + 2, :], AF.Square, ["x"], [("f1", u)])
                P.pelog.append((P.phase, "normstat", 2))
                P.op("pe", ["ones_bf", ("f1", u)], ["psS"],
                     lambda t, k2=k2, u=u: [t.matmul(psS[:, 0:T], ones_bf[:], f1b[:, u, 0, :], start=(k2 == 0), stop=False),
                                            t.matmul(psS[:, 0:T], ones_bf[:], f1b[:, u, 1, :], start=False, stop=(k2 == KD // 2 - 1))][-1])
            rstd_from(psS[:, 0:T], D, ["psS", "eps"], rbc[:])
            for k in range(KD):
                stt(f2[:, k % 2, :], x[:, k, :], gs[:, i, k:k + 1], rbc[:], ALU.mult, ALU.mult,
                    ["x", "gsgt", "rbc"], [("f2", k % 2)])
                act(h[:, k, :], f2[:, k % 2, :], AF.Identity, [("f2", k % 2), "mod"], ["h"], bias=sh(i, k))

        def accum_x(pso, dc, gi, reads):
            stt(x[:, dc, :], pso[:, 0:T], gt[:, gi, dc:dc + 1], x[:, dc, :], ALU.mult, ALU.add,
                reads + ["gsgt", "x"], ["x"])

        octr = [0]

        def ffn(fi, gi):
            norm_to_h(0 if fi == 0 else 2)
            P.phase = "ffn%d" % fi
            groups_f = [list(range(s, min(s + 4, NG))) for s in range(0, NG, 4)]

            def ab(gidx):
                mb = mid[gidx % 2]
                for li, g in enumerate(groups_f[gidx]):
                    s1, v1 = colgran(w1[fi], g * 256)
                    s3, v3 = colgran(w3[fi], g * 256)
                    for fc in range(2):
                        pa, pb = (psA, psB) if fc == 0 else (psX, psY)
                        na, nb = ("psA", "psB") if fc == 0 else ("psX", "psY")
                        mm([[(pa[:, 0:T], v1[:, k, fc * 128:(fc + 1) * 128], h[:, k, :]) for k in range(KD)]],
                           [("slot", s1), "h"], [na])
                        mm([[(pb[:, 0:T], v3[:, k, fc * 128:(fc + 1) * 128], h[:, k, :]) for k in range(KD)]],
                           [("slot", s3), "h"], [nb])
                        act(sa[fc][:], pa[:, 0:T], AF.Silu, [na], [("sa", fc)])
                        tt(mb[:, 2 * li + fc, :], sa[fc][:], pb[:, 0:T], ALU.mult, [("sa", fc), nb], [("mid", gidx % 2)])

            def down(gidx):
                mb = mid[gidx % 2]
                g0 = groups_f[gidx][0]
                nj = 2 * len(groups_f[gidx])
                for cb in range(NCB):
                    src = w2[fi].rearrange("(j p) d -> p j d", p=128)[:, 2 * g0:2 * g0 + nj, cb * NCOL:(cb + 1) * NCOL]
                    slot, wv = wload(src, NCOL, nj * NCOL)
                    for dcl in range(NCOL // 128):
                        dc = cb * (NCOL // 128) + dcl
                        o = octr[0] % 2
                        octr[0] += 1
                        mm([[(psO[o][:, 0:T], wv[:, j, dcl * 128:(dcl + 1) * 128], mb[:, j, :]) for j in range(nj)]],
                           [("slot", slot), ("mid", gidx % 2)], [("psO", o)])
                        accum_x(psO[o], dc, gi, [("psO", o)])

            ab(0)
            for gidx in range(len(groups_f)):
                if gidx + 1 < len(groups_f):
                    ab(gidx + 1)
                down(gidx)

        def wout_acc(row_chunk0):
            src = w_out.rearrange("(j p) d -> p j d", p=128)[:, row_chunk0:row_chunk0 + 2, :]
            slot, wv = wload(src, D, 2 * D)
            for dc in range(KD):
                o = octr[0] % 2
                octr[0] += 1
                mm([[(psO[o][:, 0:T], wv[:, j, dc * 128:(dc + 1) * 128], mix[:, j, :]) for j in range(2)]],
                   [("slot", slot), "mix"], [("psO", o)])
                accum_x(psO[o], dc, 1, [("psO", o)])

        def proj_fm(c0, pa, pb, na, nb):
            slot, wv = colgran(w_in, c0)
            mm([[(pa[:, 0:T], wv[:, k, 0:128], h[:, k, :]) for k in range(KD)]], [("slot", slot), "h"], [na])
            mm([[(pb[:, 0:T], wv[:, k, 128:256], h[:, k, :]) for k in range(KD)]], [("slot", slot), "h"], [nb])
            return slot, wv

        def proj_tm(slot, wv, pz, nz):
            mm([[(pz[:, tc * 256:(tc + 1) * 256], h[:, k, tc * 128:(tc + 1) * 128], wv[:, k, :]) for k in range(KD)]
                for tc in range(TC)], [("slot", slot), "h"], [nz])

        def rot_fm(pa, pb, na, nb, out, scale_bc, okey):
            tt(f1[:, 0, :], pa[:, 0:T], cosF[:], ALU.mult, [na, "Fcos"], F1K)
            tt(f1[:, 1, :], pb[:, 0:T], sinF[:], ALU.mult, [nb, "Fsin"], F1K)
            tt(f1[:, 0, :], f1[:, 0, :], f1[:, 1, :], ALU.subtract, F1K, F1K)
            tt(f2[:, 0, :], pa[:, 0:T], sinF[:], ALU.mult, [na, "Fsin"], [("f2", 0), ("f2", 1)])
            tt(f2[:, 1, :], pb[:, 0:T], cosF[:], ALU.mult, [nb, "Fcos"], [("f2", 0), ("f2", 1)])
            tt(f2[:, 0, :], f2[:, 0, :], f2[:, 1, :], ALU.add, [("f2", 0), ("f2", 1)], [("f2", 0), ("f2", 1)])
            tt(out[:, 0, :].rearrange("p (c i) -> p c i", i=128), f1[:, 0, :].rearrange("p (c i) -> p c i", i=128),
               bc(scale_bc, TC), ALU.mult, F1K + ["cst"], [okey])
            tt(out[:, 1, :].rearrange("p (c i) -> p c i", i=128), f2[:, 0, :].rearrange("p (c i) -> p c i", i=128),
               bc(scale_bc, TC), ALU.mult, [("f2", 0), ("f2", 1), "cst"], [okey])

        def ktok_rot(hh, scal):
            pz = psZ[:, 0:TC * 256].rearrange("p (c e) -> p c e", e=256)
            g1 = gv[:, :, 0:128]; g2 = gv[:, :, 128:256]
            chunks = [None] if not callable(scal) else list(range(TC))
            for c in chunks:
                cs_ = slice(None) if c is None else slice(c, c + 1)
                sc_ap = scal if c is None else scal(c)
                stt(g1[:, cs_, :], pz[:, cs_, 0:128], sc_ap, cosT[:, cs_, :], ALU.mult, ALU.mult, ["psZ", "csc", "Tcos"], ["gv"])
                stt(g2[:, cs_, :], pz[:, cs_, 128:256], sc_ap, sinT[:, cs_, :], ALU.mult, ALU.mult, ["psZ", "csc", "Tsin"], ["gv"])
                tt(ktok[:, cs_, 0:128], g1[:, cs_, :], g2[:, cs_, :], ALU.subtract, ["gv"], ["ktok"])
                stt(g1[:, cs_, :], pz[:, cs_, 0:128], sc_ap, sinT[:, cs_, :], ALU.mult, ALU.mult, ["psZ", "csc", "Tsin"], ["gv"])
                stt(g2[:, cs_, :], pz[:, cs_, 128:256], sc_ap, cosT[:, cs_, :], ALU.mult, ALU.mult, ["psZ", "csc", "Tcos"], ["gv"])
                tt(ktok[:, cs_, 128:256], g1[:, cs_, :], g2[:, cs_, :], ALU.add, ["gv"], ["ktok"])

        def mixer(t):
            norm_to_h(1)
            for hh in range(H):
                P.phase = "s1:h%d" % hh
                slot, wv = colgran(w_in, RW + hh * HD)
                proj_tm(slot, wv, psZ, "psZ")
                ktok_rot(hh, csc_sb[:, hh:hh + 1])
                for c in range(TC):
                    ts(ztok[:, c, :], ktok[:, c, :], gam[hh] ** (128 * (TC - c)), None, ALU.mult, None, ["ktok"], ["ztok"])
                slot, wv = colgran(w_in, 2 * RW + hh * HD)
                proj_tm(slot, wv, psX, "psX")
                act(vtok, psX[:, 0:TC * 256].rearrange("p (c e) -> p c e", e=256), AF.Copy, ["psX"], ["vtok"])
                dma(kvs[t * H + hh], kvt[:].rearrange("p a c e -> p (a c e)"), ["ktok", "vtok"], [("kvs", t, hh)], q="act")
                mm([[(psY[:, d * 256:(d + 1) * 256], ztok[:, c, d * 128:(d + 1) * 128], vtok[:, c, :]) for c in range(TC)]
                    for d in range(2)], ["ztok", "vtok"], ["psY"])
                Sh = S[:, hh].rearrange("p d e -> p (d e)")
                stt(cbuf[:], Sh, cm_sb[:, 2 + hh:3 + hh], psY[:, 0:512], ALU.mult, ALU.add, ["S", "cm", "psY"], ["cbuf"])
                dma(st_out[t].ap()[:, hh * 512:(hh + 1) * 512], cbuf[:], ["cbuf"], [("st_out", t)], q="act")
            P.coll([("st_out", t)], [("st_all", t)],
                   lambda g, t=t: g.collective_compute("AllGather", ALU.bypass,
                                                       replica_groups=[[2 * i, 2 * i + 1] for i in range(NPAIR)],
                                                       ins=[st_out[t].ap()[:, :]], outs=[st_all[t].ap()[:, :]]))
            for g in range(H):
                P.phase = "sgu:g%d" % g
                proj_fm(4 * RW + g * HD, psA, psB, "psA", "psB")
                act(gu[:, 0, :], psA[:, 0:T], AF.Gelu_apprx_tanh, ["psA"], ["gu"])
                act(gu[:, 1, :], psB[:, 0:T], AF.Gelu_apprx_tanh, ["psB"], ["gu"])
                slot, wv = colgran(w_in, 5 * RW + g * HD)
                proj_tm(slot, wv, psZ, "psZ")
                pz = psZ[:, 0:TC * 256].rearrange("p (c e) -> p c e", e=256)
                act(gv[:], pz, AF.Gelu_apprx_tanh, ["psZ"], ["gv"])
                P.op("dve", ["gv"], ["st4"],
                     lambda v: v.tensor_reduce(out=st4[:, 0:TC], in_=gv[:], axis=AX.X, op=ALU.add))
                act(f3[:].rearrange("p a b -> p (a b)")[:, 0:TC * 256].rearrange("p (c e) -> p c e", e=256), gv[:], AF.Square,
                    ["gv"], ["f3"])
                P.op("dve", ["f3"], ["st4b"],
                     lambda v: v.tensor_reduce(out=st4[:, 2:2 + TC],
                                               in_=f3[:].rearrange("p a b -> p (a b)")[:, 0:TC * 256].rearrange("p (c e) -> p c e", e=256),
                                               axis=AX.X, op=ALU.add))
                ts(st4[:, 0:TC], st4[:, 0:TC], 1.0 / HD, None, ALU.mult, None, ["st4"], ["st4"])
                tt(st4[:, 4:4 + TC], st4[:, 0:TC], st4[:, 0:TC], ALU.mult, ["st4"], ["st4c"])
                stt(st4[:, 2:2 + TC], st4[:, 2:2 + TC], 1.0 / HD, st4[:, 4:4 + TC], ALU.mult, ALU.subtract,
                    ["st4b", "st4c"], ["st4b"])
                act(st4[:, 2:2 + TC], st4[:, 2:2 + TC], AF.Sqrt, ["st4b", "eps"], ["st4b"], bias=eps_sb[:, 0:1])
                P.op("dve", ["st4b"], ["st4b"], lambda v: v.reciprocal(out=st4[:, 2:2 + TC], in_=st4[:, 2:2 + TC]))
                for tc in range(TC):
                    ts(ztok[:, tc, :], gv[:, tc, :], st4[:, tc:tc + 1], st4[:, 2 + tc:3 + tc], ALU.subtract, ALU.mult,
                       ["gv", "st4", "st4b"], ["ztok"])
                if g > 0:
                    wout_acc(2 * H + 2 * (g - 1))
                    P.phase = "sgu:g%d" % g
                mm([[(psX[:, d * T + tc * 128:d * T + (tc + 1) * 128], ztok[:, tc, d * 128:(d + 1) * 128], wm[:, g, :])]
                    for d in range(2) for tc in range(TC)], ["ztok", "wm"], ["psX"])
                for d in range(2):
                    stt(f3[:, d, :].rearrange("p (c i) -> p c i", i=128),
                        psX[:, d * T:(d + 1) * T].rearrange("p (c i) -> p c i", i=128),
                        gb_sb[:, 0, 2 * g + d:2 * g + d + 1], bc(T2[:, g, d, :], TC), ALU.mult, ALU.add,
                        ["psX", "gb", "T2", "f3"], ["f3"])
                tt(mix[:], f3[:], gu[:], ALU.mult, ["f3", "gu"], ["mix"])
            wout_acc(2 * H + 2 * (H - 1))


        def mixer2(t):
            for hh in range(H):
                P.phase = "s2:h%d" % hh
                Sh = S[:, hh].rearrange("p d e -> p (d e)")
                Rf = R[:].rearrange("p d e -> p (d e)")
                dma(stg[:], st_all[t].ap().rearrange("(r p) c -> p r c", p=128)[:, :, hh * 512:(hh + 1) * 512],
                    [("st_all", t)], ["stg"], q="act")
                ts(Rf, Sh, cm_sb[:, 0:1], None, ALU.mult, None, ["S", "cm"], ["R"])
                stt(Rf, stg[:, 0, :], cm_sb[:, 1:2], Rf, ALU.mult, ALU.add, ["stg", "cm", "R"], ["R"])
                stt(Sh, stg[:, 0, :], gam[hh] ** T, stg[:, 1, :], ALU.mult, ALU.add, ["stg", "S"], ["S"])
                proj_fm(hh * HD, psA, psB, "psA", "psB")
                rot_fm(psA, psB, "psA", "psB", qh, xi(hh), "qh")
                proj_fm(RW + hh * HD, psX, psY, "psX", "psY")
                rot_fm(psX, psY, "psX", "psY", kh, kz(hh), "kh")
                dma(kvt[:].rearrange("p a c e -> p (a c e)"), kvs[t * H + hh], [("kvs", t, hh)], ["ktok", "vtok"], q="act")
                proj_fm(3 * RW + hh * HD, psA, psB, "psA", "psB")
                act(sg[:, 0, :], psA[:, 0:T], AF.Silu, ["psA"], ["sg"])
                act(sg[:, 1, :], psB[:, 0:T], AF.Silu, ["psB"], ["sg"])
                if hh > 0:
                    wout_acc(2 * (hh - 1))
                    P.phase = "s2:h%d" % hh
                for c in range(TC):
                    cs = slice(c * 128, (c + 1) * 128)
                    mm([[(psX[:, 0:128], kh[:, d, cs], qh[:, d, cs]) for d in range(2)]], ["kh", "qh"], ["psX"])
                    tt(sTm[:], psX[:, 0:128], maskT, ALU.mult, ["psX", "cst"], ["sTm"])
                    cp(Sbf[:], R[:], ["R"], ["Sbf"])
                    mm([[(psY[:, ec * 128:(ec + 1) * 128], vtok[:, c, ec * 128:(ec + 1) * 128], sTm[:])] +
                        [(psY[:, ec * 128:(ec + 1) * 128], Sbf[:, d, ec * 128:(ec + 1) * 128], qh[:, d, cs]) for d in range(2)]
                        for ec in range(2)], ["vtok", "sTm", "Sbf", "qh"], ["psY"])
                    act(ret[:, :, cs], psY[:, 0:256].rearrange("p (e i) -> p e i", i=128), AF.Copy, ["psY"], ["ret"])
                    if c < TC - 1:
                        mm([[(psZ[:, d * 256:(d + 1) * 256], ktok[:, c, d * 128:(d + 1) * 128], vtok[:, c, :])] for d in range(2)],
                           ["ktok", "vtok"], ["psZ"])
                        ts(Rf, Rf, gam[hh] ** 128, None, ALU.mult, None, ["R"], ["R"])
                        stt(Rf, psZ[:, 0:512], gam[hh] ** 128, Rf, ALU.mult, ALU.add, ["psZ", "R"], ["R"])
                act(f1b[:, 0], ret[:], AF.Square, ["ret"], [("f1", 0)])
                mm([[(psS[:, 0:T], ones_bf[:], f1b[:, 0, 0, :]), (psS[:, 0:T], ones_bf[:], f1b[:, 0, 1, :])]],
                   ["ones_bf", ("f1", 0)], ["psS"])
                rstd_from(psS[:, 0:T], HD, ["psS", "eps"], rbc[:])
                tt(f3[:], ret[:], bc(rbc[:], 2), ALU.mult, ["ret", "rbc", "f3"], ["f3"])
                tt(mix[:], f3[:], sg[:], ALU.mult, ["f3", "sg"], ["mix"])
            wout_acc(2 * (H - 1))

        for t in range(NT):
            tsl = slice(t * T, (t + 1) * T)
            cache_mode[0] = "fill" if t == 0 else "use"
            gctr[0] = 0
            dma(x[:], xT.rearrange("(k p) s -> p k s", p=128)[:, :, tsl], [], ["x"])
            dma(posI[:], posb[:, tsl], [], ["posI"])
            cp(posF[:], posI[:], ["posI"], ["posF"])
            ts(f1[:, 0, :], posF[:], csc_sb[:, H:H + 1], None, ALU.mult, None, ["posF", "csc"], F1K)
            sincos(f1[:, 0, :], sinF[:], cosF[:], f2[:, 0, :], f2[:, 1, :], ki[:], "F", F1K, [("f2", 0)], [("f2", 1)])
            for tc in range(TC):
                ts(f3[:, 0, tc * 128:(tc + 1) * 128], invf_bc, posTf[:, t * TC + tc:t * TC + tc + 1], None, ALU.mult, None,
                   ["cst", "posTf"], ["f3"])
            sincos(f3[:, 0, :], sinT[:].rearrange("p c f -> p (c f)"), cosT[:].rearrange("p c f -> p (c f)"),
                   f3[:, 1, :], gv[:, 0, :], ki[:], "T", ["f3"], ["f3"], ["gv"])
            ffn(0, 0)
            mixer(t)
            mixer2(t)
            ffn(1, 2)
            P.phase = "final"
            for k2 in range(KD // 2):
                u = k2 % 2
                act(f1b[:, u], x[:, 2 * k2:2 * k2 + 2, :], AF.Square, ["x"], [("f1", u)])
                P.pelog.append((P.phase, "normstat", 2))
                P.op("pe", ["ones_bf", ("f1", u)], ["psS"],
                     lambda t, k2=k2, u=u: [t.matmul(psS[:, 0:T], ones_bf[:], f1b[:, u, 0, :], start=(k2 == 0), stop=False),
                                            t.matmul(psS[:, 0:T], ones_bf[:], f1b[:, u, 1, :], start=False, stop=(k2 == KD // 2 - 1))][-1])
            rstd_from(psS[:, 0:T], D, ["psS", "eps"], rbc[:])
            for k in range(KD):
                stt(x[:, k, :], x[:, k, :], gains_sb[:, 3, k:k + 1], rbc[:], ALU.mult, ALU.mult, ["x", "gains", "rbc"], ["x"])
            dma(outT.rearrange("(k p) s -> p k s", p=128)[:, :, tsl], x[:], ["x"], ["out"])
            assert gctr[0] == NGR, (gctr[0], NGR)

        names = ["pe", "act", "dve", "sp", "cc", "pd", "ad"] + ["w%d" % i for i in range(NB)] + ["b%d" % i for i in range(NB)]
        semmap = {}
        for n in names:
            semmap[n] = es.enter_context(nc.semaphore("s_" + n))
        last_sp = 16 * P.cnt["sp"]
        block = es.enter_context(nc.Block())
        P.emit(nc, block, semmap)
    return nc, P


def _consts(H):
    gam = np.array([1.0 - 2.0 ** (-5.0 - h) for h in range(H)], np.float64)
    i = np.arange(128, dtype=np.float64)
    cst = np.zeros((128, 2 * H + 3, 128), np.float32)
    for h in range(H):
        cst[:, h, :] = (gam[h] ** (i + 1))[None, :]
        cst[:, H + h, :] = (gam[h] ** (-(i + 1)) * HD ** -0.5)[None, :]
    cst[:, 2 * H, :] = (i[None, :] >= i[:, None]).astype(np.float32)
    invf = (10000.0 ** (-(np.arange(128, dtype=np.float32) / np.float32(128)))).astype(np.float32)
    cst[:, 2 * H + 1, :] = invf[None, :]
    TC = T // CH
    csc = np.zeros((128, H + 1 + H * TC), np.float32)
    for h in range(H):
        csc[:, h] = gam[h] ** (-(i + 1)) * HD ** -0.5
        for c in range(TC):
            csc[:, H + 1 + h * TC + c] = gam[h] ** (-(i + 1)) * HD ** -0.5 * gam[h] ** (128 * (TC - c))
    csc[:, H] = invf
    return cst, csc


def fm(v):
    return np.ascontiguousarray(np.asarray(v).reshape(-1, 128).T)


def _tiles(seq, half):
    nt = seq // (2 * T)
    return np.concatenate([np.arange((2 * j + half) * T, (2 * j + half + 1) * T) for j in range(nt)])


def make_inputs(r, x, c, positions, shared, H):
    b, half = r // 2, r % 2
    m = dict(shared)
    idx = _tiles(x.shape[1], half)
    m["xT"] = np.ascontiguousarray(np.asarray(x[b])[idx].T)
    m["cT"] = fm(c[b])
    pos = np.asarray(positions[b]).astype(np.int32)[idx]
    m["posb"] = np.ascontiguousarray(np.broadcast_to(pos[None, :], (128, pos.shape[0])))
    m["post"] = fm(pos)
    NBt = c.shape[0]
    CW = shared["_ada_w"].shape[1] // 2
    m["ada_w"] = np.ascontiguousarray(shared["_ada_w"][:, half * CW:(half + 1) * CW])
    m["ada_bT"] = fm(shared["_ada_b"][half * CW:(half + 1) * CW])
    del m["_ada_w"], m["_ada_b"]
    cm = np.zeros((128, 2 + H), np.float32)
    cm[:, 0] = 1.0 - half
    cm[:, 1] = float(half)
    for h in range(H):
        cm[:, 2 + h] = (1.0 - half) * (1.0 - 2.0 ** (-5.0 - h)) ** T
    m["cm"] = cm
    return m


def kernel(**inp):
    cfg = inp.pop("_cfg", None) or dict(D=4096, DFF=11008, H=8, SEQ=4096, BATCH=4)
    H = cfg["H"]
    a = {k: np.asarray(v) for k, v in inp.items()}
    cst, csc = _consts(H)
    shared = {
        "_ada_w": a["ada_w"][0], "_ada_b": a["ada_b"][0],
        "gains": np.ascontiguousarray(np.stack([fm(a["norm_ffn1_g"][0]), fm(a["norm_mix_g"][0]),
                                                fm(a["norm_ffn2_g"][0]), fm(a["final_norm_g"])], axis=1)),
        "f1w1": a["ffn1_w1"][0], "f1w3": a["ffn1_w3"][0], "f1w2": a["ffn1_w2"][0],
        "f2w1": a["ffn2_w1"][0], "f2w3": a["ffn2_w3"][0], "f2w2": a["ffn2_w2"][0],
        "w_in": a["w_in"][0], "w_out": a["w_out"][0],
        "sgu_gb": np.ascontiguousarray(np.stack([fm(a["sgu_norm_g"][0].reshape(-1)), fm(a["sgu_norm_b"][0].reshape(-1))], axis=1)),
        "wsT": np.ascontiguousarray(a["sgu_w_s"][0].transpose(2, 0, 1)),
        "bsb": np.ascontiguousarray(np.broadcast_to(a["sgu_b_s"][0][None], (128, H, 128))),
        "cst": cst, "csc": csc,
    }
    B = cfg["BATCH"]
    SEQ = cfg["SEQ"]
    nc, _ = build(cfg)
    in_maps = [make_inputs(r, a["x"], a["c"], a["positions"], shared, H) for r in range(2 * B)]
    res = run_bass_kernel_spmd(nc, in_maps, core_ids=list(range(2 * B)))
    out = np.zeros((B, SEQ, cfg["D"]), np.float32)
    for r in range(2 * B):
        out[r // 2, _tiles(SEQ, r % 2)] = res.results[r]["outT"].T
    return out
```

```python
import math
import numpy as np
import concourse.bass as bass
import concourse.mybir as mybir
from concourse.bass_utils import run_bass_kernel_spmd

F32 = mybir.dt.float32
BF16 = mybir.dt.bfloat16
I32 = mybir.dt.int32
AF = mybir.ActivationFunctionType
ALU = mybir.AluOpType
AX = mybir.AxisListType

EPS = 1e-6
HD = 256
CH = 128
T = 256
NB = 4
SLOT = 8192


class Prog:
    def __init__(self):
        self.q = {e: [] for e in ("pe", "act", "dve", "pool", "sp")}
        self.cnt = {e: 0 for e in self.q}
        self.res = {}
        self.waited = {e: {} for e in self.q}
        self.slot_cnt = [0] * NB
        self.sems = {}
        self.pelog = []
        self.phase = "setup"

    def _deps(self, eng, reads, writes):
        toks = []
        for r in reads:
            st = self.res.get(r)
            if st and st["w"] is not None:
                toks.append(st["w"])
        for w in writes:
            st = self.res.get(w)
            if st:
                if st["w"] is not None and not (st["w"][0] == eng and st["w"][1] == eng):
                    toks.append(st["w"])
                for t in st["r"]:
                    if not (t[0] == eng and t[1] == eng):
                        toks.append(t)
        need = {}
        for (e, sem, val) in toks:
            if e == eng and eng == "pe":
                continue
            if need.get(sem, 0) < val:
                need[sem] = val
        out = []
        for sem, val in need.items():
            if self.waited[eng].get(sem, 0) < val:
                self.waited[eng][sem] = val
                out.append((sem, val))
        return out

    def _commit(self, tok, reads, writes):
        for r in reads:
            st = self.res.setdefault(r, {"w": None, "r": []})
            st["r"].append(tok)
        for w in writes:
            self.res[w] = {"w": tok, "r": []}

    def op(self, eng, reads, writes, fn):
        waits = self._deps(eng, reads, writes)
        self.cnt[eng] += 1
        if eng in ("pe", "act", "dve"):
            sem, val, inc = eng, self.cnt[eng], 1
        elif eng == "sp":
            sem, val, inc = "sp", 16 * self.cnt[eng], 16
            if self.cnt[eng] > 1:
                prev = 16 * (self.cnt[eng] - 1)
                if self.waited[eng].get("sp", 0) < prev:
                    self.waited[eng]["sp"] = prev
                    waits.append(("sp", prev))
        elif eng == "pool":
            self.npd = getattr(self, "npd", 0) + 1
            sem, val, inc = "pd", 16 * self.npd, 16
            if self.npd > 1:
                prev = 16 * (self.npd - 1)
                if self.waited[eng].get("pd", 0) < prev:
                    self.waited[eng]["pd"] = prev
                    waits.append(("pd", prev))
        else:
            raise ValueError(eng)
        tok = (eng, sem, val)
        self.q[eng].append((waits, fn, sem, inc))
        self._commit(tok, reads, writes)

    def qdma(self, queue, reads, writes, fn):
        sem = {"sp": "sp", "act": "ad", "pool": "pd"}[queue]
        waits = self._deps(queue, reads, writes)
        self.ndma = getattr(self, "ndma", {})
        n = self.ndma.get(queue, 0) + 1
        self.ndma[queue] = n
        if n > 1:
            prev = 16 * (n - 1)
            if self.waited[queue].get(sem, 0) < prev:
                self.waited[queue][sem] = prev
                waits.append((sem, prev))
        tok = (queue, sem, 16 * n)
        self.q[queue].append((waits, fn, sem, 16))
        self._commit(tok, reads, writes)

    def spw_load(self, slot, reads, fn):
        eng = "sp"
        key = ("slot", slot)
        waits = self._deps(eng, reads, [key])
        self.slot_cnt[slot] += 1
        sem = "w%d" % slot
        tok = (eng, sem, 16 * self.slot_cnt[slot])
        self.q[eng].append((waits, fn, sem, 16))
        self._commit(tok, reads, [key])

    def spw_store(self, slot, writes, fn):
        eng = "sp"
        key = ("slot", slot)
        waits = self._deps(eng, [key], writes)
        self.bcnt = getattr(self, "bcnt", [0] * NB)
        self.bcnt[slot] += 1
        sem = "b%d" % slot
        tok = (eng, sem, 16 * self.bcnt[slot])
        self.q[eng].append((waits, fn, sem, 16))
        self._commit(tok, [key], writes)

    def wload(self, slot, fns):
        eng = "pool"
        key = ("slot", slot)
        waits = self._deps(eng, [], [key])
        sem = "w%d" % slot
        for i, fn in enumerate(fns):
            self.slot_cnt[slot] += 1
            self.q[eng].append((waits if i == 0 else [], fn, sem, 16))
        tok = (eng, sem, 16 * self.slot_cnt[slot])
        self._commit(tok, [], [key])

    def coll(self, reads, writes, fn):
        eng = "pool"
        waits = self._deps(eng, reads, writes)
        self.ncc = getattr(self, "ncc", 0) + 1
        tok = (eng, "cc", self.ncc)
        self.q[eng].append((waits, fn, "cc", 1))
        self._commit(tok, reads, writes)

    def emit(self, nc, block, semmap):
        engs = {"pe": "tensor", "act": "scalar", "dve": "vector", "pool": "gpsimd", "sp": "sync"}
        for e, name in engs.items():
            ops = self.q[e]

            def body(eng, ops=ops, e=e):
                for waits, fn, sem, inc in ops:
                    for (s, v) in waits:
                        eng.wait_ge(semmap[s], v)
                    ins = fn(eng)
                    ins.then_inc(semmap[sem], inc)
                if e == "sp":
                    eng.wait_ge(semmap["sp"], 16 * getattr(self, "ndma", {}).get("sp", 0))
            getattr(block, name)(body)


def build(cfg):
    D, DFF, H, SEQ = cfg["D"], cfg["DFF"], cfg["H"], cfg["SEQ"]
    KD = D // 128
    RW = H * HD
    INW = 6 * RW
    NT = SEQ // (2 * T)
    TC = T // CH
    NG = DFF // 256
    NF = DFF // 128
    NCOL = min(D, 1024)
    NCB = D // NCOL
    gam = [1.0 - 2.0 ** (-5.0 - h) for h in range(H)]

    nc = bass.Bass("TRN2", target_bir_lowering=False)

    def din(name, shape, dt=F32):
        return nc.dram_tensor(name, list(shape), dt, kind="ExternalInput").ap()

    xT = din("xT", [D, SEQ // 2])
    NCORE = 2 * cfg["BATCH"]
    NB_ = cfg["BATCH"]
    CW = 9 * D // 2
    NCHL = CW // 128
    cT = din("cT", [128, KD])
    posb = din("posb", [128, SEQ // 2], I32)
    post = din("post", [128, SEQ // 256], I32)
    ada_w = din("ada_w", [D, CW])
    ada_bT = din("ada_bT", [128, NCHL])
    gains = din("gains", [128, 4, KD])
    w1 = [din("f1w1", [D, DFF]), din("f2w1", [D, DFF])]
    w3 = [din("f1w3", [D, DFF]), din("f2w3", [D, DFF])]
    w2 = [din("f1w2", [DFF, D]), din("f2w2", [DFF, D])]
    w_in = din("w_in", [D, INW])
    w_out = din("w_out", [D, D])
    sgu_gb = din("sgu_gb", [128, 2, 2 * H])
    wsT = din("wsT", [128, H, 128])
    bsb = din("bsb", [128, H, 128])
    cst = din("cst", [128, 2 * H + 3, 128])
    csc = din("csc", [128, H + 1 + H * TC])
    cm = din("cm", [128, 2 + H])
    outT = nc.dram_tensor("outT", [D, SEQ // 2], F32, kind="ExternalOutput").ap()

    NPAIR = cfg["BATCH"]
    st_out = [nc.dram_tensor("st_out%d" % i, [128, H * 512], F32) for i in range(NT)]
    st_all = [nc.dram_tensor("st_all%d" % i, [256, H * 512], F32) for i in range(NT)]
    m_out = nc.dram_tensor("m_out", [128, NCHL], F32)
    m_all = nc.dram_tensor("m_all", [2 * 128, NCHL], F32)
    kvs = nc.dram_tensor("kvs", [NT * H, 128, 2 * TC * HD], BF16).ap()
    NGR = 2 * (2 * NG + ((NG + 3) // 4) * NCB) + 9 * H
    WCN = 96
    wcs = [nc.dram_tensor("wcache%d" % i, [WCN, 128, SLOT], BF16).ap() for i in range((NGR + WCN - 1) // WCN)]
    wcg = lambda gi: wcs[gi // WCN][gi % WCN]
    import os
    dummies = [nc.dram_tensor("dummy%d" % i, [WCN, 128, SLOT], BF16).ap() for i in range(int(os.environ.get("DUMMY_SCRATCH", "0")))]
    P = Prog()
    import contextlib
    es = contextlib.ExitStack()

    def sb(name, shape, dt=F32):
        return es.enter_context(nc.sbuf_tensor(name, list(shape), dt))

    def ps(name):
        return es.enter_context(nc.psum_tensor(name, [128, 512], F32))

    with es:
        x = sb("x", [128, KD, T])
        h = sb("h", [128, KD, T], BF16)
        ring = [sb("ring%d" % i, [128, SLOT], BF16) for i in range(NB)]
        mid = [sb("mid%d" % i, [128, 8, T], BF16) for i in range(2)]
        S = sb("S", [128, H, 2, HD])
        Sbf = sb("Sbf", [128, 2, HD], BF16)
        qh = sb("qh", [128, 2, T], BF16)
        kh = sb("kh", [128, 2, T], BF16)
        kvt = sb("kvt", [128, 2, TC, HD], BF16)
        ktok = kvt[:, 0]
        vtok = kvt[:, 1]
        sg = sb("sg", [128, 2, T], BF16)
        gu = sb("gu", [128, 2, T], BF16)
        sTm = sb("sTm", [128, 128], BF16)
        ret = sb("ret", [128, 2, T])
        mix = sb("mix", [128, 2, T], BF16)
        gv = sb("gv", [128, TC, HD])
        ztok = sb("ztok", [128, TC, HD], BF16)
        f1 = sb("f1", [128, 2, T])
        f2 = sb("f2", [128, 2, T])
        f1b = f1[:].rearrange("p a b -> p (a b)").bitcast(BF16).rearrange("p (u c t) -> p u c t", u=2, c=2)
        F1K = [("f1", 0), ("f1", 1)]
        f3 = sb("f3", [128, 2, T])
        sa = [sb("sa%d" % i, [128, T]) for i in range(2)]
        rbc = sb("rbc", [128, T])
        st4 = sb("st4", [128, 8])
        cosF = sb("cosF", [128, T]); sinF = sb("sinF", [128, T])
        cosT = sb("cosT", [128, TC, 128]); sinT = sb("sinT", [128, TC, 128])
        posF = sb("posF", [128, T]); posI = sb("posI", [128, T], I32)
        posTi = sb("posTi", [128, SEQ // 256], I32); posTf = sb("posTf", [128, SEQ // 256])
        ki = sb("ki", [128, T], I32)
        cst_sb = sb("cst_sb", [128, 2 * H + 3, 128])
        csc_sb = sb("csc_sb", [128, H + 1 + H * TC])
        cm_sb = sb("cm_sb", [128, 2 + H])
        R = sb("R", [128, 2, HD])
        cbuf = sb("cbuf", [128, 2 * HD])
        stg = sb("stg", [128, 2, 2 * HD])
        wsf = stg[:].rearrange("p a b -> p (a b)")[:, 0:H * 128].rearrange("p (g i) -> p g i", i=128)
        wm = sb("wm", [128, H, 128], BF16)
        bs_sb = sb("bs_sb", [128, H, 128])
        T2 = sb("T2", [128, H, 2, 128])
        gb_sb = sb("gb_sb", [128, 2, 2 * H])
        gains_sb = sb("gains_sb", [128, 4, KD])
        c_sb = sb("c_sb", [128, KD]); c_bf = sb("c_bf", [128, KD], BF16)
        adab = sb("adab", [128, NCHL])
        mod_loc = sb("mod_loc", [128, NCHL])
        mod = sb("mod", [128, 9 * KD])
        gs = sb("gs", [128, 3, KD])
        gt = sb("gt", [128, 3, KD])
        ones = sb("ones", [128, 128])
        psA = ps("psA"); psB = ps("psB"); psX = ps("psX"); psY = ps("psY")
        psO = [ps("psO0"), ps("psO1")]; psS = ps("psS"); psZ = ps("psZ")

        xi = lambda hh: cst_sb[:, hh, :]
        kz = lambda hh: cst_sb[:, H + hh, :]
        maskT = cst_sb[:, 2 * H, :]
        invf_bc = cst_sb[:, 2 * H + 1, :]

        def bc(ap, n):
            return ap.unsqueeze(1).to_broadcast([128, n, ap.shape[-1]])

        wctr = [0]
        gctr = [0]
        cache_mode = [None]

        def wload(src, n_inner, total):
            slot = wctr[0] % NB
            wctr[0] += 1
            dst = ring[slot][:, 0:total].rearrange("p (k f) -> p k f", f=n_inner)
            if cache_mode[0] is not None:
                gi = gctr[0]
                gctr[0] += 1
                assert gi < NGR
                if cache_mode[0] == "use":
                    P.spw_load(slot, [("wc", gi)],
                               lambda e, gi=gi, slot=slot, total=total: e.dma_start(out=ring[slot][:, 0:total], in_=wcg(gi)[:, 0:total]))
                    return slot, dst
            if n_inner * 4 > 4096:
                fns = [(lambda g, d=dst[:, j, :], s_=src[:, j, :]: g.dma_start(out=d, in_=s_, max_dma_last_dim=4096))
                       for j in range(total // n_inner)]
            else:
                fns = [lambda g, dst=dst, src=src: g.dma_start(out=dst, in_=src, max_dma_last_dim=4096)]
            P.wload(slot, fns)
            if cache_mode[0] == "fill":
                P.spw_store(slot, [("wc", gi)],
                            lambda e, gi=gi, slot=slot, total=total: e.dma_start(out=wcg(gi)[:, 0:total], in_=ring[slot][:, 0:total]))
            return slot, dst

        def colgran(w, c0):
            return wload(w.rearrange("(k p) f -> p k f", p=128)[:, :, c0:c0 + 256], 256, KD * 256)

        def dma(dst, src, reads, writes, q="sp"):
            P.qdma(q, reads, writes, lambda s: s.dma_start(out=dst, in_=src))

        def tt(out, a, b, op, reads, writes, eng="dve"):
            P.op(eng, reads, writes, lambda v: v.tensor_tensor(out=out, in0=a, in1=b, op=op))

        def ts(out, a, s1, s2, op0, op1, reads, writes):
            if op1 is None:
                P.op("dve", reads, writes, lambda v: v.tensor_scalar(out=out, in0=a, scalar1=s1, scalar2=None, op0=op0))
            else:
                P.op("dve", reads, writes, lambda v: v.tensor_scalar(out=out, in0=a, scalar1=s1, scalar2=s2, op0=op0, op1=op1))

        def stt(out, a, s, b, op0, op1, reads, writes):
            P.op("dve", reads, writes, lambda v: v.scalar_tensor_tensor(out=out, in0=a, scalar=s, in1=b, op0=op0, op1=op1))

        def act(out, a, func, reads, writes, bias=None, scale=None):
            kw = {}
            if bias is not None:
                kw["bias"] = bias
            if scale is not None:
                kw["scale"] = scale
            P.op("act", reads, writes, lambda e: e.activation(out=out, in_=a, func=func, **kw))

        def cp(out, a, reads, writes):
            P.op("dve", reads, writes, lambda v: v.tensor_copy(out=out, in_=a))

        def mm(groups, reads, writes):
            P.pelog.append((P.phase, str(writes), sum(len(g) for g in groups) * (2 if groups[0][0][1].dtype == F32 else 1)))
            def fn(t):
                ins = None
                for g in groups:
                    n = len(g)
                    for i, (o, l, r) in enumerate(g):
                        ins = t.matmul(o, l, r, start=(i == 0), stop=(i == n - 1))
                return ins
            P.op("pe", reads, writes, fn)

        def rstd_from(psum_ap, n, reads, out):
            act(out, psum_ap, AF.Sqrt, reads, ["rbc"], bias=eps_sb[:, 0:1], scale=1.0 / n)
            P.op("dve", ["rbc"], ["rbc"], lambda v: v.reciprocal(out=out, in_=out))

        eps_sb = sb("eps_sb", [128, 1])

        P.op("dve", [], ["ones"], lambda v: v.memset(ones[:], 1.0))
        P.op("dve", [], ["eps"], lambda v: v.memset(eps_sb[:], EPS))
        P.op("dve", [], ["S"], lambda v: v.memset(S[:], 0.0))
        dma(c_sb[:], cT, [], ["c_sb"])
        dma(adab[:], ada_bT, [], ["adab"])
        dma(gains_sb[:], gains, [], ["gains"])
        dma(gb_sb[:], sgu_gb, [], ["gb"])
        dma(wsf, wsT, [], ["stg"])
        dma(bs_sb[:], bsb, [], ["bs"])
        dma(cst_sb[:], cst, [], ["cst"])
        dma(csc_sb[:], csc, [], ["csc"])
        dma(cm_sb[:], cm, [], ["cm"])
        dma(posTi[:], post, [], ["posTi"])
        cp(posTf[:], posTi[:], ["posTi"], ["posTf"])
        act(c_bf[:], c_sb[:], AF.Silu, ["c_sb"], ["c_bf"])
        tt(wm[:], wsf, bc(maskT, H), ALU.mult, ["stg", "cst"], ["wm"])
        ones_bf = sb("ones_bf", [128, 128], BF16)
        P.op("dve", [], ["ones_bf"], lambda v: v.memset(ones_bf[:], 1.0))
        for g in range(H):
            mm([[(psS[:, 0:128], ones_bf[:], wm[:, g, :])]], ["ones_bf", "wm"], ["psS"])
            for dch in range(2):
                stt(T2[:, g, dch, :], psS[:, 0:128], gb_sb[:, 1, 2 * g + dch:2 * g + dch + 1], bs_sb[:, g, :],
                    ALU.mult, ALU.add, ["psS", "gb", "bs"], ["T2"])
        for g in range(CW // 256):
            slot, wv = colgran(ada_w, g * 256)
            groups = [[(psZ[:, 2 * g + j:2 * g + j + 1], wv[:, k, j * 128:(j + 1) * 128], c_bf[:, k:k + 1])
                       for k in range(KD)] for j in range(2)]
            mm(groups, [("slot", slot), "c_bf"], ["psZ"])
        tt(mod_loc[:], psZ[:, 0:NCHL], adab[:], ALU.add, ["psZ", "adab"], ["mod_loc"])
        dma(m_out.ap()[:, :], mod_loc[:], ["mod_loc"], ["m_out"])
        P.coll(["m_out"], ["m_all"],
               lambda g: g.collective_compute("AllGather", ALU.bypass,
                                              replica_groups=[[2 * i, 2 * i + 1] for i in range(NPAIR)],
                                              ins=[m_out.ap()[:, :]], outs=[m_all.ap()[:, :]]))
        dma(mod[:].rearrange("p (r c) -> p r c", c=NCHL), m_all.ap().rearrange("(r p) f -> p r f", p=128), ["m_all"], ["mod"])
        for i in range(3):
            stt(gs[:, i, :], mod[:, (3 * i + 1) * KD:(3 * i + 2) * KD], 1.0, gains_sb[:, i, :], ALU.add, ALU.mult,
                ["mod", "gains"], ["gsgt"])
            ts(gt[:, i, :], mod[:, (3 * i + 2) * KD:(3 * i + 3) * KD], 0.5 if i != 1 else 1.0, None, ALU.mult, None,
               ["mod"], ["gsgt"])
        sh = lambda i, k: mod[:, 3 * i * KD + k:3 * i * KD + k + 1]

        def sincos(ang, sin_o, cos_o, tmpa, tmpb, kint, key, angk, ak, bk):
            C1 = 6.28125
            C2 = 2.0 * math.pi - C1
            ts(tmpa, ang, 1.0 / (2.0 * math.pi), None, ALU.mult, None, angk, ak)
            cp(kint, tmpa, ak, ["kint"])
            cp(tmpa, kint, ["kint"], ak)
            stt(tmpb, tmpa, -C1, ang, ALU.mult, ALU.add, ak + angk, bk)
            stt(tmpb, tmpa, -C2, tmpb, ALU.mult, ALU.add, ak + bk, bk)
            ts(tmpb, tmpb, math.pi, -math.pi, ALU.min, ALU.max, bk, bk)
            act(sin_o, tmpb, AF.Sin, bk, [key + "sin"])
            ts(tmpa, tmpb, math.pi / 2, None, ALU.add, None, bk, ak)
            ts(tmpb, tmpa, math.pi, -2.0 * math.pi, ALU.is_gt, ALU.mult, ak, bk)
            tt(tmpa, tmpa, tmpb, ALU.add, ak + bk, ak)
            ts(tmpa, tmpa, math.pi, -math.pi, ALU.min, ALU.max, ak, ak)
            act(cos_o, tmpa, AF.Sin, ak, [key + "cos"])

        def norm_to_h(i):
            P.phase = "norm%d" % i
            for k2 in range(KD // 2):
                u = k2 % 2
                act(f1b[:, u], x[:, 2 * k2:2 * k2 + 2, :], AF.Square, ["x"], [("f1", u)])
                P.pelog.append((P.phase, "normstat", 2))
                P.op("pe", ["ones_bf", ("f1", u)], ["psS"],
                     lambda t, k2=k2, u=u: [t.matmul(psS[:, 0:T], ones_bf[:], f1b[:, u, 0, :], start=(k2 == 0), stop=False),
                                            t.matmul(psS[:, 0:T], ones_bf[:], f1b[:, u, 1, :], start=False, stop=(k2 == KD // 2 - 1))][-1])
            rstd_from(psS[:, 0:T], D, ["psS", "eps"], rbc[:])
            stage = [(f2[:, 0, :], ("f2", 0)), (f2[:, 1, :], ("f2", 1)), (f3[:, 0, :], "f3"), (gv[:, 0, :], "gv")]
            for k in range(KD):
                sbuf_, skey = stage[k % 4]
                stt(sbuf_, x[:, k, :], gs[:, i, k:k + 1], rbc[:], ALU.mult, ALU.mult,
                    ["x", "gsgt", "rbc"], [skey])
                act(h[:, k, :], sbuf_, AF.Identity, [skey, "mod"], ["h"], bias=sh(i, k))

        def accum_x(pso, dc, gi, reads):
            stt(x[:, dc, :], pso[:, 0:T], gt[:, gi, dc:dc + 1], x[:, dc, :], ALU.mult, ALU.add,
                reads + ["gsgt", "x"], ["x"])

        octr = [0]

        def ffn(fi, gi, hook=None):
            norm_to_h(0 if fi == 0 else 2)
            if hook is not None:
                hook()
            P.phase = "ffn%d" % fi
            groups_f = [list(range(s, min(s + 4, NG))) for s in range(0, NG, 4)]

            def ab(gidx):
                mb = mid[gidx % 2]
                for li, g in enumerate(groups_f[gidx]):
                    s1, v1 = colgran(w1[fi], g * 256)
                    s3, v3 = colgran(w3[fi], g * 256)
                    for fc in range(2):
                        pa, pb = (psA, psB) if fc == 0 else (psX, psY)
                        na, nb = ("psA", "psB") if fc == 0 else ("psX", "psY")
                        mm([[(pa[:, 0:T], v1[:, k, fc * 128:(fc + 1) * 128], h[:, k, :]) for k in range(KD)]],
                           [("slot", s1), "h"], [na])
                        mm([[(pb[:, 0:T], v3[:, k, fc * 128:(fc + 1) * 128], h[:, k, :]) for k in range(KD)]],
                           [("slot", s3), "h"], [nb])
                        act(sa[fc][:], pa[:, 0:T], AF.Silu, [na], [("sa", fc)])
                        tt(mb[:, 2 * li + fc, :], sa[fc][:], pb[:, 0:T], ALU.mult, [("sa", fc), nb], [("mid", gidx % 2)])

            def down(gidx):
                mb = mid[gidx % 2]
                g0 = groups_f[gidx][0]
                nj = 2 * len(groups_f[gidx])
                for cb in range(NCB):
                    src = w2[fi].rearrange("(j p) d -> p j d", p=128)[:, 2 * g0:2 * g0 + nj, cb * NCOL:(cb + 1) * NCOL]
                    slot, wv = wload(src, NCOL, nj * NCOL)
                    for dcl in range(NCOL // 128):
                        dc = cb * (NCOL // 128) + dcl
                        o = octr[0] % 2
                        octr[0] += 1
                        mm([[(psO[o][:, 0:T], wv[:, j, dcl * 128:(dcl + 1) * 128], mb[:, j, :]) for j in range(nj)]],
                           [("slot", slot), ("mid", gidx % 2)], [("psO", o)])
                        accum_x(psO[o], dc, gi, [("psO", o)])

            ab(0)
            for gidx in range(len(groups_f)):
                if gidx + 1 < len(groups_f):
                    ab(gidx + 1)
                down(gidx)

        def wout_acc(row_chunk0):
            src = w_out.rearrange("(j p) d -> p j d", p=128)[:, row_chunk0:row_chunk0 + 2, :]
            slot, wv = wload(src, D, 2 * D)
            for dc in range(KD):
                o = octr[0] % 2
                octr[0] += 1
                mm([[(psO[o][:, 0:T], wv[:, j, dc * 128:(dc + 1) * 128], mix[:, j, :]) for j in range(2)]],
                   [("slot", slot), "mix"], [("psO", o)])
                accum_x(psO[o], dc, 1, [("psO", o)])

        def proj_fm(c0, pa, pb, na, nb):
            slot, wv = colgran(w_in, c0)
            mm([[(pa[:, 0:T], wv[:, k, 0:128], h[:, k, :]) for k in range(KD)]], [("slot", slot), "h"], [na])
            mm([[(pb[:, 0:T], wv[:, k, 128:256], h[:, k, :]) for k in range(KD)]], [("slot", slot), "h"], [nb])
            return slot, wv

        def proj_tm(slot, wv, pz, nz):
            mm([[(pz[:, tc * 256:(tc + 1) * 256], h[:, k, tc * 128:(tc + 1) * 128], wv[:, k, :]) for k in range(KD)]
                for tc in range(TC)], [("slot", slot), "h"], [nz])

        def rot_fm(pa, pb, na, nb, out, scale_bc, okey):
            tt(f1[:, 0, :], pa[:, 0:T], cosF[:], ALU.mult, [na, "Fcos"], F1K)
            tt(f1[:, 1, :], pb[:, 0:T], sinF[:], ALU.mult, [nb, "Fsin"], F1K)
            tt(f1[:, 0, :], f1[:, 0, :], f1[:, 1, :], ALU.subtract, F1K, F1K)
            tt(f2[:, 0, :], pa[:, 0:T], sinF[:], ALU.mult, [na, "Fsin"], [("f2", 0), ("f2", 1)])
            tt(f2[:, 1, :], pb[:, 0:T], cosF[:], ALU.mult, [nb, "Fcos"], [("f2", 0), ("f2", 1)])
            tt(f2[:, 0, :], f2[:, 0, :], f2[:, 1, :], ALU.add, [("f2", 0), ("f2", 1)], [("f2", 0), ("f2", 1)])
            tt(out[:, 0, :].rearrange("p (c i) -> p c i", i=128), f1[:, 0, :].rearrange("p (c i) -> p c i", i=128),
               bc(scale_bc, TC), ALU.mult, F1K + ["cst"], [okey])
            tt(out[:, 1, :].rearrange("p (c i) -> p c i", i=128), f2[:, 0, :].rearrange("p (c i) -> p c i", i=128),
               bc(scale_bc, TC), ALU.mult, [("f2", 0), ("f2", 1), "cst"], [okey])

        def ktok_rot(hh, scal):
            pz = psZ[:, 0:TC * 256].rearrange("p (c e) -> p c e", e=256)
            g1 = gv[:, :, 0:128]; g2 = gv[:, :, 128:256]
            chunks = [None] if not callable(scal) else list(range(TC))
            for c in chunks:
                cs_ = slice(None) if c is None else slice(c, c + 1)
                sc_ap = scal if c is None else scal(c)
                stt(g1[:, cs_, :], pz[:, cs_, 0:128], sc_ap, cosT[:, cs_, :], ALU.mult, ALU.mult, ["psZ", "csc", "Tcos"], ["gv"])
                stt(g2[:, cs_, :], pz[:, cs_, 128:256], sc_ap, sinT[:, cs_, :], ALU.mult, ALU.mult, ["psZ", "csc", "Tsin"], ["gv"])
                tt(ktok[:, cs_, 0:128], g1[:, cs_, :], g2[:, cs_, :], ALU.subtract, ["gv"], ["ktok"])
                stt(g1[:, cs_, :], pz[:, cs_, 0:128], sc_ap, sinT[:, cs_, :], ALU.mult, ALU.mult, ["psZ", "csc", "Tsin"], ["gv"])
                stt(g2[:, cs_, :], pz[:, cs_, 128:256], sc_ap, cosT[:, cs_, :], ALU.mult, ALU.mult, ["psZ", "csc", "Tcos"], ["gv"])
                tt(ktok[:, cs_, 128:256], g1[:, cs_, :], g2[:, cs_, :], ALU.add, ["gv"], ["ktok"])

        def mixer(t):
            norm_to_h(1)
            for hh in range(H):
                P.phase = "s1:h%d" % hh
                slot, wv = colgran(w_in, RW + hh * HD)
                proj_tm(slot, wv, psZ, "psZ")
                ktok_rot(hh, csc_sb[:, hh:hh + 1])
                for c in range(TC):
                    ts(ztok[:, c, :], ktok[:, c, :], gam[hh] ** (128 * (TC - c)), None, ALU.mult, None, ["ktok"], ["ztok"])
                slot, wv = colgran(w_in, 2 * RW + hh * HD)
                proj_tm(slot, wv, psX, "psX")
                act(vtok, psX[:, 0:TC * 256].rearrange("p (c e) -> p c e", e=256), AF.Copy, ["psX"], ["vtok"])
                dma(kvs[t * H + hh], kvt[:].rearrange("p a c e -> p (a c e)"), ["ktok", "vtok"], [("kvs", t, hh)], q="act")
                mm([[(psY[:, d * 256:(d + 1) * 256], ztok[:, c, d * 128:(d + 1) * 128], vtok[:, c, :]) for c in range(TC)]
                    for d in range(2)], ["ztok", "vtok"], ["psY"])
                Sh = S[:, hh].rearrange("p d e -> p (d e)")
                stt(cbuf[:], Sh, cm_sb[:, 2 + hh:3 + hh], psY[:, 0:512], ALU.mult, ALU.add, ["S", "cm", "psY"], ["cbuf"])
                dma(st_out[t].ap()[:, hh * 512:(hh + 1) * 512], cbuf[:], ["cbuf"], [("st_out", t)], q="act")
            P.coll([("st_out", t)], [("st_all", t)],
                   lambda g, t=t: g.collective_compute("AllGather", ALU.bypass,
                                                       replica_groups=[[2 * i, 2 * i + 1] for i in range(NPAIR)],
                                                       ins=[st_out[t].ap()[:, :]], outs=[st_all[t].ap()[:, :]]))
            for g in range(H):
                P.phase = "sgu:g%d" % g
                slot, wv = colgran(w_in, 5 * RW + g * HD)
                proj_tm(slot, wv, psZ, "psZ")
                pz = psZ[:, 0:TC * 256].rearrange("p (c e) -> p c e", e=256)
                act(gv[:], pz, AF.Gelu_apprx_tanh, ["psZ"], ["gv"])
                P.op("dve", ["gv"], ["st4"],
                     lambda v: v.tensor_reduce(out=st4[:, 0:TC], in_=gv[:], axis=AX.X, op=ALU.add))
                act(f3[:].rearrange("p a b -> p (a b)")[:, 0:TC * 256].rearrange("p (c e) -> p c e", e=256), gv[:], AF.Square,
                    ["gv"], ["f3"])
                P.op("dve", ["f3"], ["st4b"],
                     lambda v: v.tensor_reduce(out=st4[:, 2:2 + TC],
                                               in_=f3[:].rearrange("p a b -> p (a b)")[:, 0:TC * 256].rearrange("p (c e) -> p c e", e=256),
                                               axis=AX.X, op=ALU.add))
                ts(st4[:, 0:TC], st4[:, 0:TC], 1.0 / HD, None, ALU.mult, None, ["st4"], ["st4"])
                tt(st4[:, 4:4 + TC], st4[:, 0:TC], st4[:, 0:TC], ALU.mult, ["st4"], ["st4c"])
                stt(st4[:, 2:2 + TC], st4[:, 2:2 + TC], 1.0 / HD, st4[:, 4:4 + TC], ALU.mult, ALU.subtract,
                    ["st4b", "st4c"], ["st4b"])
                act(st4[:, 2:2 + TC], st4[:, 2:2 + TC], AF.Sqrt, ["st4b", "eps"], ["st4b"], bias=eps_sb[:, 0:1])
                P.op("dve", ["st4b"], ["st4b"], lambda v: v.reciprocal(out=st4[:, 2:2 + TC], in_=st4[:, 2:2 + TC]))
                for tc in range(TC):
                    ts(ztok[:, tc, :], gv[:, tc, :], st4[:, tc:tc + 1], st4[:, 2 + tc:3 + tc], ALU.subtract, ALU.mult,
                       ["gv", "st4", "st4b"], ["ztok"])
                proj_fm(4 * RW + g * HD, psA, psB, "psA", "psB")
                act(gu[:, 0, :], psA[:, 0:T], AF.Gelu_apprx_tanh, ["psA"], ["gu"])
                act(gu[:, 1, :], psB[:, 0:T], AF.Gelu_apprx_tanh, ["psB"], ["gu"])
                if g > 0:
                    wout_acc(2 * H + 2 * (g - 1))
                    P.phase = "sgu:g%d" % g
                mm([[(psX[:, d * T + tc * 128:d * T + (tc + 1) * 128], ztok[:, tc, d * 128:(d + 1) * 128], wm[:, g, :])]
                    for d in range(2) for tc in range(TC)], ["ztok", "wm"], ["psX"])
                for d in range(2):
                    stt(f3[:, d, :].rearrange("p (c i) -> p c i", i=128),
                        psX[:, d * T:(d + 1) * T].rearrange("p (c i) -> p c i", i=128),
                        gb_sb[:, 0, 2 * g + d:2 * g + d + 1], bc(T2[:, g, d, :], TC), ALU.mult, ALU.add,
                        ["psX", "gb", "T2", "f3"], ["f3"])
                tt(mix[:], f3[:], gu[:], ALU.mult, ["f3", "gu"], ["mix"])
            wout_acc(2 * H + 2 * (H - 1))


        def mixer2(t):
            for hh in range(H):
                P.phase = "s2:h%d" % hh
                Sh = S[:, hh].rearrange("p d e -> p (d e)")
                Rf = R[:].rearrange("p d e -> p (d e)")
                dma(stg[:], st_all[t].ap().rearrange("(r p) c -> p r c", p=128)[:, :, hh * 512:(hh + 1) * 512],
                    [("st_all", t)], ["stg"], q="act")
                ts(Rf, Sh, cm_sb[:, 0:1], None, ALU.mult, None, ["S", "cm"], ["R"])
                stt(Rf, stg[:, 0, :], cm_sb[:, 1:2], Rf, ALU.mult, ALU.add, ["stg", "cm", "R"], ["R"])
                stt(Sh, stg[:, 0, :], gam[hh] ** T, stg[:, 1, :], ALU.mult, ALU.add, ["stg", "S"], ["S"])
                proj_fm(hh * HD, psA, psB, "psA", "psB")
                rot_fm(psA, psB, "psA", "psB", qh, xi(hh), "qh")
                proj_fm(RW + hh * HD, psX, psY, "psX", "psY")
                rot_fm(psX, psY, "psX", "psY", kh, kz(hh), "kh")
                dma(kvt[:].rearrange("p a c e -> p (a c e)"), kvs[t * H + hh], [("kvs", t, hh)], ["ktok", "vtok"], q="act")
                proj_fm(3 * RW + hh * HD, psA, psB, "psA", "psB")
                act(sg[:, 0, :], psA[:, 0:T], AF.Silu, ["psA"], ["sg"])
                act(sg[:, 1, :], psB[:, 0:T], AF.Silu, ["psB"], ["sg"])
                if hh > 0:
                    wout_acc(2 * (hh - 1))
                    P.phase = "s2:h%d" % hh
                for c in range(TC):
                    cs = slice(c * 128, (c + 1) * 128)
                    mm([[(psX[:, 0:128], kh[:, d, cs], qh[:, d, cs]) for d in range(2)]], ["kh", "qh"], ["psX"])
                    tt(sTm[:], psX[:, 0:128], maskT, ALU.mult, ["psX", "cst"], ["sTm"])
                    cp(Sbf[:], R[:], ["R"], ["Sbf"])
                    mm([[(psY[:, ec * 128:(ec + 1) * 128], vtok[:, c, ec * 128:(ec + 1) * 128], sTm[:])] +
                        [(psY[:, ec * 128:(ec + 1) * 128], Sbf[:, d, ec * 128:(ec + 1) * 128], qh[:, d, cs]) for d in range(2)]
                        for ec in range(2)], ["vtok", "sTm", "Sbf", "qh"], ["psY"])
                    act(ret[:, :, cs], psY[:, 0:256].rearrange("p (e i) -> p e i", i=128), AF.Copy, ["psY"], ["ret"])
                    if c < TC - 1:
                        mm([[(psZ[:, d * 256:(d + 1) * 256], ktok[:, c, d * 128:(d + 1) * 128], vtok[:, c, :])] for d in range(2)],
                           ["ktok", "vtok"], ["psZ"])
                        ts(Rf, Rf, gam[hh] ** 128, None, ALU.mult, None, ["R"], ["R"])
                        stt(Rf, psZ[:, 0:512], gam[hh] ** 128, Rf, ALU.mult, ALU.add, ["psZ", "R"], ["R"])
                act(f1b[:, 0], ret[:], AF.Square, ["ret"], [("f1", 0)])
                mm([[(psS[:, 0:T], ones_bf[:], f1b[:, 0, 0, :]), (psS[:, 0:T], ones_bf[:], f1b[:, 0, 1, :])]],
                   ["ones_bf", ("f1", 0)], ["psS"])
                rstd_from(psS[:, 0:T], HD, ["psS", "eps"], rbc[:])
                tt(f3[:], ret[:], bc(rbc[:], 2), ALU.mult, ["ret", "rbc", "f3"], ["f3"])
                tt(mix[:], f3[:], sg[:], ALU.mult, ["f3", "sg"], ["mix"])
            wout_acc(2 * (H - 1))

        for t in range(NT):
            tsl = slice(t * T, (t + 1) * T)
            cache_mode[0] = "fill" if t == 0 else "use"
            gctr[0] = 0
            dma(x[:], xT.rearrange("(k p) s -> p k s", p=128)[:, :, tsl], [], ["x"])
            dma(posI[:], posb[:, tsl], [], ["posI"])
            def rot_tables(t=t):
                cp(posF[:], posI[:], ["posI"], ["posF"])
                ts(f1[:, 0, :], posF[:], csc_sb[:, H:H + 1], None, ALU.mult, None, ["posF", "csc"], F1K)
                sincos(f1[:, 0, :], sinF[:], cosF[:], f2[:, 0, :], f2[:, 1, :], ki[:], "F", F1K, [("f2", 0)], [("f2", 1)])
                for tc in range(TC):
                    ts(f3[:, 0, tc * 128:(tc + 1) * 128], invf_bc, posTf[:, t * TC + tc:t * TC + tc + 1], None, ALU.mult, None,
                       ["cst", "posTf"], ["f3"])
                sincos(f3[:, 0, :], sinT[:].rearrange("p c f -> p (c f)"), cosT[:].rearrange("p c f -> p (c f)"),
                       f3[:, 1, :], gv[:, 0, :], ki[:], "T", ["f3"], ["f3"], ["gv"])
            ffn(0, 0, hook=rot_tables)
            mixer(t)
            mixer2(t)
            ffn(1, 2)
            P.phase = "final"
            for k2 in range(KD // 2):
                u = k2 % 2
                act(f1b[:, u], x[:, 2 * k2:2 * k2 + 2, :], AF.Square, ["x"], [("f1", u)])
                P.pelog.append((P.phase, "normstat", 2))
                P.op("pe", ["ones_bf", ("f1", u)], ["psS"],
                     lambda t, k2=k2, u=u: [t.matmul(psS[:, 0:T], ones_bf[:], f1b[:, u, 0, :], start=(k2 == 0), stop=False),
                                            t.matmul(psS[:, 0:T], ones_bf[:], f1b[:, u, 1, :], start=False, stop=(k2 == KD // 2 - 1))][-1])
            rstd_from(psS[:, 0:T], D, ["psS", "eps"], rbc[:])
            for k in range(KD):
                stt(x[:, k, :], x[:, k, :], gains_sb[:, 3, k:k + 1], rbc[:], ALU.mult, ALU.mult, ["x", "gains", "rbc"], ["x"])
            dma(outT.rearrange("(k p) s -> p k s", p=128)[:, :, tsl], x[:], ["x"], ["out"])
            assert gctr[0] == NGR, (gctr[0], NGR)

        names = ["pe", "act", "dve", "sp", "cc", "pd", "ad"] + ["w%d" % i for i in range(NB)] + ["b%d" % i for i in range(NB)]
        semmap = {}
        for n in names:
            semmap[n] = es.enter_context(nc.semaphore("s_" + n))
        last_sp = 16 * P.cnt["sp"]
        block = es.enter_context(nc.Block())
        P.emit(nc, block, semmap)
    return nc, P


def _consts(H):
    gam = np.array([1.0 - 2.0 ** (-5.0 - h) for h in range(H)], np.float64)
    i = np.arange(128, dtype=np.float64)
    cst = np.zeros((128, 2 * H + 3, 128), np.float32)
    for h in range(H):
        cst[:, h, :] = (gam[h] ** (i + 1))[None, :]
        cst[:, H + h, :] = (gam[h] ** (-(i + 1)) * HD ** -0.5)[None, :]
    cst[:, 2 * H, :] = (i[None, :] >= i[:, None]).astype(np.float32)
    invf = (10000.0 ** (-(np.arange(128, dtype=np.float32) / np.float32(128)))).astype(np.float32)
    cst[:, 2 * H + 1, :] = invf[None, :]
    TC = T // CH
    csc = np.zeros((128, H + 1 + H * TC), np.float32)
    for h in range(H):
        csc[:, h] = gam[h] ** (-(i + 1)) * HD ** -0.5
        for c in range(TC):
            csc[:, H + 1 + h * TC + c] = gam[h] ** (-(i + 1)) * HD ** -0.5 * gam[h] ** (128 * (TC - c))
    csc[:, H] = invf
    return cst, csc


def fm(v):
    return np.ascontiguousarray(np.asarray(v).reshape(-1, 128).T)


def _tiles(seq, half):
    nt = seq // (2 * T)
    return np.concatenate([np.arange((2 * j + half) * T, (2 * j + half + 1) * T) for j in range(nt)])


def make_inputs(r, x, c, positions, shared, H):
    b, half = r // 2, r % 2
    m = dict(shared)
    idx = _tiles(x.shape[1], half)
    m["xT"] = np.ascontiguousarray(np.asarray(x[b])[idx].T)
    m["cT"] = fm(c[b])
    pos = np.asarray(positions[b]).astype(np.int32)[idx]
    m["posb"] = np.ascontiguousarray(np.broadcast_to(pos[None, :], (128, pos.shape[0])))
    m["post"] = fm(pos)
    NBt = c.shape[0]
    CW = shared["_ada_w"].shape[1] // 2
    m["ada_w"] = np.ascontiguousarray(shared["_ada_w"][:, half * CW:(half + 1) * CW])
    m["ada_bT"] = fm(shared["_ada_b"][half * CW:(half + 1) * CW])
    del m["_ada_w"], m["_ada_b"]
    cm = np.zeros((128, 2 + H), np.float32)
    cm[:, 0] = 1.0 - half
    cm[:, 1] = float(half)
    for h in range(H):
        cm[:, 2 + h] = (1.0 - half) * (1.0 - 2.0 ** (-5.0 - h)) ** T
    m["cm"] = cm
    return m


def kernel(**inp):
    cfg = inp.pop("_cfg", None) or dict(D=4096, DFF=11008, H=8, SEQ=4096, BATCH=4)
    H = cfg["H"]
    a = {k: np.asarray(v) for k, v in inp.items()}
    cst, csc = _consts(H)
    shared = {
        "_ada_w": a["ada_w"][0], "_ada_b": a["ada_b"][0],
        "gains": np.ascontiguousarray(np.stack([fm(a["norm_ffn1_g"][0]), fm(a["norm_mix_g"][0]),
                                                fm(a["norm_ffn2_g"][0]), fm(a["final_norm_g"])], axis=1)),
        "f1w1": a["ffn1_w1"][0], "f1w3": a["ffn1_w3"][0], "f1w2": a["ffn1_w2"][0],
        "f2w1": a["ffn2_w1"][0], "f2w3": a["ffn2_w3"][0], "f2w2": a["ffn2_w2"][0],
        "w_in": a["w_in"][0], "w_out": a["w_out"][0],
        "sgu_gb": np.ascontiguousarray(np.stack([fm(a["sgu_norm_g"][0].reshape(-1)), fm(a["sgu_norm_b"][0].reshape(-1))], axis=1)),
        "wsT": np.ascontiguousarray(a["sgu_w_s"][0].transpose(2, 0, 1)),
        "bsb": np.ascontiguousarray(np.broadcast_to(a["sgu_b_s"][0][None], (128, H, 128))),
        "cst": cst, "csc": csc,
    }
    B = cfg["BATCH"]
    SEQ = cfg["SEQ"]
    nc, _ = build(cfg)
    in_maps = [make_inputs(r, a["x"], a["c"], a["positions"], shared, H) for r in range(2 * B)]
    res = run_bass_kernel_spmd(nc, in_maps, core_ids=list(range(2 * B)))
    out = np.zeros((B, SEQ, cfg["D"]), np.float32)
    for r in range(2 * B):
        out[r // 2, _tiles(SEQ, r % 2)] = res.results[r]["outT"].T
    return out
```
